# Optimizing a Trainium2 kernel written in Bass

```python
import math
import jax, jax.numpy as jnp
from jax import lax
import numpy as np

D_MODEL = 1024
BATCH = 8
SEQ = 4096
DEPTH = 1

PLE_DIM = 256
D_FF = 2816
EPS = 1e-6
ATT_HEADS = 8
ATT_HEAD_DIM = 64
KV_LATENT = 128
IDX_HEADS = 8
IDX_HEAD_DIM = 64
TOPK_MAX = 256
Q_BLOCK = 128
ATT_SCALE = ATT_HEAD_DIM ** -0.5
IDX_W_SCALE = (IDX_HEADS ** -0.5) * (IDX_HEAD_DIM ** -0.5)
REL_BUCKETS = 32
REL_MAX_DIST = 128
HG_HEADS = 4
HG_KEY_DIM = 128
HG_VAL_DIM = 128
CHUNK = 64
ATT_WIDTH = ATT_HEADS * ATT_HEAD_DIM
IDX_Q_WIDTH = IDX_HEADS * IDX_HEAD_DIM
HG_KW = HG_HEADS * HG_KEY_DIM
HG_VW = HG_HEADS * HG_VAL_DIM
IN_SPLITS = (ATT_WIDTH, KV_LATENT, IDX_Q_WIDTH, IDX_HEAD_DIM, IDX_HEADS, HG_KW, HG_KW, HG_VW, HG_VW, D_MODEL, D_MODEL)
N_IN = sum(IN_SPLITS)

kernel_name = 'hybrid_dsa_hgrn2_macaron'


def rms_norm(x, g):
    xf = x.astype(jnp.float32)
    y = xf * lax.rsqrt(jnp.mean(xf * xf, axis=-1, keepdims=True) + EPS)
    return (y * g.astype(jnp.float32)).astype(x.dtype)


def swiglu(x, w_gate, w_up, w_down):
    return (jax.nn.silu(x @ w_gate) * (x @ w_up)) @ w_down


def split_cols(a):
    outs = []
    off = 0
    for w in IN_SPLITS:
        outs.append(a[..., off:off + w])
        off += w
    return outs


def rel_bucket(dist):
    max_exact = REL_BUCKETS // 2
    d_f = jnp.maximum(dist, 1).astype(jnp.float32)
    log_b = max_exact + (jnp.log(d_f / max_exact) / math.log(REL_MAX_DIST / max_exact)
                         * (REL_BUCKETS - max_exact)).astype(jnp.int32)
    return jnp.where(dist < max_exact, dist, jnp.minimum(log_b, REL_BUCKETS - 1))


def dsa_branch(q, c_kv, q_idx, k_idx, w_idx, w_uk, w_uv, rel_bias):
    B_, S_ = q.shape[0], q.shape[1]
    topk = min(TOPK_MAX, S_ // 4)
    nblk = S_ // Q_BLOCK
    q_lat = jnp.einsum('bshd,hcd->bshc', q, w_uk)
    key_pos = jnp.arange(S_, dtype=jnp.int32)

    def to_blocks(a):
        return jnp.moveaxis(a.reshape((B_, nblk, Q_BLOCK) + a.shape[2:]), 1, 0)

    def block(args):
        ql, qi, wi, t_pos = args
        dots = jnp.einsum('bqhd,bsd->bqhs', qi, k_idx)
        score = jnp.einsum('bqh,bqhs->bqs', wi, jax.nn.relu(dots)).astype(jnp.float32)
        causal = key_pos[None, :] <= t_pos[:, None]
        score = jnp.where(causal[None], score, -jnp.inf)
        _, idx = lax.top_k(score, topk)
        valid = idx <= t_pos[None, :, None]
        c_sel = jax.vmap(lambda c, i: c[i])(c_kv, idx)
        dist = jnp.maximum(t_pos[None, :, None] - idx, 0)
        bias = jnp.moveaxis(rel_bias[rel_bucket(dist)], -1, 2)
        logits = (jnp.einsum('bqhc,bqkc->bqhk', ql.astype(jnp.float32), c_sel.astype(jnp.float32)) * ATT_SCALE
                  + bias.astype(jnp.float32))
        logits = jnp.where(valid[:, :, None, :], logits, -jnp.inf)
        probs = jax.nn.softmax(logits, axis=-1).astype(c_kv.dtype)
        return jnp.einsum('bqhk,bqkc->bqhc', probs, c_sel)

    t_blocks = jnp.arange(S_, dtype=jnp.int32).reshape(nblk, Q_BLOCK)
    o_lat = lax.map(block, (to_blocks(q_lat), to_blocks(q_idx), to_blocks(w_idx), t_blocks))
    o_lat = jnp.moveaxis(o_lat, 0, 1).reshape(B_, S_, ATT_HEADS, KV_LATENT)
    o = jnp.einsum('bshc,hcd->bshd', o_lat, w_uv)
    return o.reshape(B_, S_, ATT_WIDTH)


def hgrn2_chunkwise(q, k, v, log_f):
    B_, S_, H_, DK_ = q.shape
    DV_ = v.shape[-1]
    n = S_ // CHUNK

    def chunks(a):
        return jnp.moveaxis(a.astype(jnp.float32).reshape(B_, n, CHUNK, H_, a.shape[-1]), 1, 0)

    tri = jnp.tril(jnp.ones((CHUNK, CHUNK), dtype=bool))

    def step(state, inp):
        qc, kc, vc, gc = inp
        G = jnp.cumsum(gc, axis=1)
        o_inter = jnp.einsum('bthk,bhkv->bthv', qc * jnp.exp(G), state)
        diff = G[:, :, None] - G[:, None, :]
        decay = jnp.exp(jnp.where(tri[None, :, :, None, None], diff, -jnp.inf))
        A = jnp.einsum('bthk,bshk,btshk->bhts', qc, kc, decay)
        o_intra = jnp.einsum('bhts,bshv->bthv', A, vc)
        G_last = G[:, -1]
        k_dec = kc * jnp.exp(G_last[:, None] - G)
        new_state = jnp.exp(G_last)[..., None] * state + jnp.einsum('bshk,bshv->bhkv', k_dec, vc)
        return new_state, o_inter + o_intra

    s0 = jnp.zeros((B_, H_, DK_, DV_), jnp.float32)
    _, o = lax.scan(step, s0, (chunks(q), chunks(k), chunks(v), chunks(log_f)))
    return jnp.moveaxis(o, 0, 1).reshape(B_, S_, H_, DV_)


def setup_inputs(seed: int = 0) -> dict:
    key = jax.random.key(seed)
    ks = jax.random.split(key, 40)

    def nrm(k, shape, scale):
        return jax.random.normal(k, shape, jnp.float32) * scale

    def gain(k, shape):
        return 1.0 + 0.05 * jax.random.normal(k, shape, jnp.float32)

    L = DEPTH
    D = D_MODEL
    return {
        'x': nrm(ks[0], (BATCH, SEQ, D), 1.0),
        'p': nrm(ks[1], (DEPTH, BATCH, SEQ, PLE_DIM), 1.0),
        'rel_bias': nrm(ks[2], (REL_BUCKETS, ATT_HEADS), 0.1),
        'hgrn_lb': nrm(ks[3], (DEPTH + 1, HG_KW), 1.0),
        'ffn1_pre_g': gain(ks[4], (L, D)),
        'ffn1_post_g': gain(ks[5], (L, D)),
        'ffn1_w_gate': nrm(ks[6], (L, D, D_FF), D ** -0.5),
        'ffn1_w_up': nrm(ks[7], (L, D, D_FF), D ** -0.5),
        'ffn1_w_down': nrm(ks[8], (L, D_FF, D), D_FF ** -0.5),
        'mix_pre_g': gain(ks[9], (L, D)),
        'mix_post_g': gain(ks[10], (L, D)),
        'w_in': nrm(ks[11], (L, D, N_IN), D ** -0.5),
        'kv_norm_g': gain(ks[12], (L, KV_LATENT)),
        'kidx_norm_g': gain(ks[13], (L, IDX_HEAD_DIM)),
        'w_uk': nrm(ks[14], (L, ATT_HEADS, KV_LATENT, ATT_HEAD_DIM), KV_LATENT ** -0.5),
        'w_uv': nrm(ks[15], (L, ATT_HEADS, KV_LATENT, ATT_HEAD_DIM), KV_LATENT ** -0.5),
        'hgrn_norm_g': gain(ks[16], (L, HG_VAL_DIM)),
        'w_br_a': nrm(ks[17], (L, ATT_WIDTH, D), ATT_WIDTH ** -0.5),
        'w_br_b': nrm(ks[18], (L, HG_VW, D), HG_VW ** -0.5),
        'b_gate': nrm(ks[19], (L, 2 * D), 0.02),
        'w_out': nrm(ks[20], (L, D, D), D ** -0.5),
        'ffn2_pre_g': gain(ks[21], (L, D)),
        'ffn2_post_g': gain(ks[22], (L, D)),
        'ffn2_w_gate': nrm(ks[23], (L, D, D_FF), D ** -0.5),
        'ffn2_w_up': nrm(ks[24], (L, D, D_FF), D ** -0.5),
        'ffn2_w_down': nrm(ks[25], (L, D_FF, D), D_FF ** -0.5),
        'ple_pre_g': gain(ks[26], (L, D)),
        'ple_post_g': gain(ks[27], (L, D)),
        'w_ple_gate': nrm(ks[28], (L, D, D), D ** -0.5),
        'w_ple_proj': nrm(ks[29], (L, PLE_DIM, D), PLE_DIM ** -0.5),
    }


def reference(x, p, rel_bias, hgrn_lb, ffn1_pre_g, ffn1_post_g, ffn1_w_gate, ffn1_w_up, ffn1_w_down,
              mix_pre_g, mix_post_g, w_in, kv_norm_g, kidx_norm_g, w_uk, w_uv, hgrn_norm_g,
              w_br_a, w_br_b, b_gate, w_out, ffn2_pre_g, ffn2_post_g, ffn2_w_gate, ffn2_w_up, ffn2_w_down,
              ple_pre_g, ple_post_g, w_ple_gate, w_ple_proj):
    B_, S_, D = x.shape
    lower_bounds = jnp.cumsum(jax.nn.softmax(hgrn_lb.astype(jnp.float32), axis=0), axis=0)
    h = x
    for i in range(DEPTH):
        h = h + 0.5 * rms_norm(swiglu(rms_norm(h, ffn1_pre_g[i]), ffn1_w_gate[i], ffn1_w_up[i], ffn1_w_down[i]),
                               ffn1_post_g[i])
        u = rms_norm(h, mix_pre_g[i])
        proj = u @ w_in[i]
        (q_a, c_kv, q_idx, k_idx, w_idx, q_h, f_h, i_h, g_h, gate_a, gate_b) = split_cols(proj)
        c_kv = rms_norm(c_kv, kv_norm_g[i])
        k_idx = rms_norm(k_idx, kidx_norm_g[i])
        y_a = dsa_branch(q_a.reshape(B_, S_, ATT_HEADS, ATT_HEAD_DIM), c_kv,
                         q_idx.reshape(B_, S_, IDX_HEADS, IDX_HEAD_DIM), k_idx, w_idx * IDX_W_SCALE,
                         w_uk[i], w_uv[i], rel_bias)
        lb = lower_bounds[i]
        f = (lb + (1.0 - lb) * jax.nn.sigmoid(f_h.astype(jnp.float32))).reshape(B_, S_, HG_HEADS, HG_KEY_DIM)
        o_h = hgrn2_chunkwise(jax.nn.silu(q_h).reshape(B_, S_, HG_HEADS, HG_KEY_DIM), 1.0 - f,
                              i_h.reshape(B_, S_, HG_HEADS, HG_VAL_DIM), jnp.log(f))
        o_h = rms_norm(o_h, hgrn_norm_g[i]) * jax.nn.silu(g_h.astype(jnp.float32).reshape(B_, S_, HG_HEADS, HG_VAL_DIM))
        y_b = o_h.reshape(B_, S_, HG_VW).astype(x.dtype)
        merged = (jax.nn.sigmoid(gate_a + b_gate[i, :D]) * (y_a @ w_br_a[i])
                  + jax.nn.sigmoid(gate_b + b_gate[i, D:]) * (y_b @ w_br_b[i]))
        h = h + rms_norm(merged @ w_out[i], mix_post_g[i])
        h = h + 0.5 * rms_norm(swiglu(rms_norm(h, ffn2_pre_g[i]), ffn2_w_gate[i], ffn2_w_up[i], ffn2_w_down[i]),
                               ffn2_post_g[i])
        ple_gate = jax.nn.sigmoid(rms_norm(h, ple_pre_g[i]) @ w_ple_gate[i])
        h = h + rms_norm(ple_gate * (p[i] @ w_ple_proj[i]), ple_post_g[i])
    return h
```

```python
import math
import numpy as np
import concourse.bass as bass
import concourse.mybir as mybir
from concourse.bass_utils import run_bass_kernel_spmd

F32 = mybir.dt.float32
BF16 = mybir.dt.bfloat16
ALU = mybir.AluOpType
AF = mybir.ActivationFunctionType
AX = mybir.AxisListType

NDMA_RING = 8
SEQ = 4096
D = 1024
DFF = 2816
NFC = 22
EPS = 1e-6
NT = 32
NM = 8
TOPK = 256
IDX_W_SCALE = (8 ** -0.5) * (64 ** -0.5)
ATT_SCALE = 64 ** -0.5
MASKV = 30000.0
ZEPS = 2.0 ** -10


class Sched:
    ENG = ("pe", "act", "dve", "pool", "sp")

    def __init__(self, nc):
        self.nc = nc
        self.stream = {e: [] for e in self.ENG}
        self.cnt = {e: 0 for e in self.ENG}
        self.pending = {e: False for e in self.ENG}
        self.seen = {e: {} for e in self.ENG}
        self.lastw = {}
        self.readers = {}
        self.dma_n = {}
        self.dma_slot_cnt = {}
        self.sems = {}
        self.srcs = set()

    def _need(self, deps, r, w):
        for k in r:
            lw = self.lastw.get(k)
            if lw is not None:
                deps[lw[0]] = max(deps.get(lw[0], 0), lw[1])
        for k in w:
            lw = self.lastw.get(k)
            if lw is not None:
                deps[lw[0]] = max(deps.get(lw[0], 0), lw[1])
            for (s, v) in self.readers.get(k, ()):
                deps[s] = max(deps.get(s, 0), v)

    def _emit_waits(self, eng, deps):
        for s, v in deps.items():
            if eng == "pe" and s == ("e", "pe"):
                continue
            if self.seen[eng].get(s, 0) >= v:
                continue
            self.seen[eng][s] = v
            self.stream[eng].append(("wait", s, v))

    def _record(self, src, val, r, w):
        for k in r:
            self.readers.setdefault(k, []).append((src, val))
        for k in w:
            self.lastw[k] = (src, val)
            self.readers[k] = []

    def op(self, eng, fn, r=(), w=(), inc=True):
        deps = {}
        self._need(deps, r, w)
        self._emit_waits(eng, deps)
        src = ("e", eng)
        self.srcs.add(src)
        if inc:
            self.cnt[eng] += 1
            val = self.cnt[eng]
            self.pending[eng] = False
            self.stream[eng].append(("inst", fn, src, 1))
        else:
            val = self.cnt[eng] + 1
            self.pending[eng] = True
            self.stream[eng].append(("inst", fn, None, 0))
        self._record(src, val, r, w)

    def dma(self, q, fn, r=(), w=()):
        deps = {}
        self._need(deps, r, w)
        n = self.dma_n.get(q, 0)
        self.dma_n[q] = n + 1
        src = ("d", q, n % NDMA_RING)
        self.srcs.add(src)
        c = self.dma_slot_cnt.get(src, 0)
        if c > 0:
            deps[src] = max(deps.get(src, 0), 16 * c)
        self._emit_waits(q, deps)
        c += 1
        self.dma_slot_cnt[src] = c
        self.stream[q].append(("inst", fn, src, 16))
        self._record(src, 16 * c, r, w)

    def _all_latest(self):
        deps = {}
        for e in ("pe", "act", "dve", "pool"):
            if self.cnt[e] > 0:
                deps[("e", e)] = self.cnt[e]
        for src, c in self.dma_slot_cnt.items():
            deps[src] = 16 * c
        return deps

    def barrier(self):
        for e in self.ENG:
            assert not self.pending[e]
        deps = self._all_latest()
        for e in self.ENG:
            self._emit_waits(e, dict(deps))

    def finish(self, eng="sp"):
        self._emit_waits(eng, self._all_latest())

    def emit(self):
        nc = self.nc
        from contextlib import ExitStack
        with ExitStack() as es:
            for s in sorted(self.srcs):
                self.sems[s] = es.enter_context(nc.semaphore("s_" + "_".join(map(str, s))))
            for e in self.ENG:
                assert not self.pending[e], e
            block = es.enter_context(nc.Block())
            reg = {"pe": block.tensor, "act": block.scalar, "dve": block.vector,
                   "pool": block.gpsimd, "sp": block.sync}

            def mk(e):
                def body(h):
                    for it in self.stream[e]:
                        if it[0] == "wait":
                            h.wait_ge(self.sems[it[1]], it[2])
                        else:
                            ins = it[1](h)
                            if it[2] is not None:
                                ins.then_inc(self.sems[it[2]], it[3])
                return body
            for e in self.ENG:
                if self.stream[e]:
                    reg[e](mk(e))


def I(name, *a, **k):
    return lambda h: getattr(h, name)(*a, **k)


def _bucket_table():
    d = np.arange(0, 512, dtype=np.int32)
    max_exact = 16
    d_f = np.maximum(d, 1).astype(np.float32)
    log_b = max_exact + (np.log(d_f / np.float32(max_exact)) / np.float32(math.log(128 / max_exact))
                         * np.float32(32 - max_exact)).astype(np.int32)
    return np.where(d < max_exact, d, np.minimum(log_b, 31))


def host_consts():
    c = {}
    c["c_ident"] = np.eye(128, dtype=np.float32)
    c["c_anti"] = np.ascontiguousarray(np.eye(128, dtype=np.float32)[::-1])
    t = np.arange(128)[:, None]
    s = np.arange(128)[None, :]
    c["c_cmneg"] = np.where(s <= t, 0.0, -MASKV).astype(np.float32)
    c["c_cmpos"] = np.where(s <= t, 0.0, MASKV).astype(np.float32)
    c["c_tri"] = (((t // 64) == (s // 64)) & (t <= s)).astype(np.float32)
    bt = _bucket_table()
    oh = np.zeros((32, 384), np.float32)
    for m in range(384):
        dd = 255 - m
        if dd >= 0:
            oh[bt[dd], m] += 1.0
            oh[31, m] -= 1.0
    c["c_oh"] = oh
    nit = 32
    c["c_halves"] = np.tile((0.5 ** np.arange(1, nit + 1, dtype=np.float64)).astype(np.float32)[None, :], (128, 1))
    return c


def host_weights(inp):
    w = {}
    for n, pre in (("wffn1", "ffn1"), ("wffn2", "ffn2")):
        wg = inp[pre + "_w_gate"][0].reshape(8, 128, NFC, 128).transpose(2, 1, 0, 3).reshape(NFC, 128, 1024)
        wu = inp[pre + "_w_up"][0].reshape(8, 128, NFC, 128).transpose(2, 1, 0, 3).reshape(NFC, 128, 1024)
        wd = inp[pre + "_w_down"][0].reshape(NFC, 128, 1024)
        w[n] = np.ascontiguousarray(np.concatenate([wg, wu, wd], axis=2))
    win = inp["w_in"][0]
    offs = np.cumsum([0, 512, 128, 512, 64, 8, 512, 512, 512, 512, 1024, 1024])
    sl = lambda k: win[:, offs[k]:offs[k + 1]]
    q_a, c_kv, q_idx, k_idx, w_idx, q_h, f_h, i_h, g_h, gate_a, gate_b = [sl(k) for k in range(11)]
    fm = np.concatenate([q_a, q_idx, q_h, f_h, g_h, gate_a, gate_b], axis=1)
    nch = fm.shape[1] // 128
    w["win_fm"] = np.ascontiguousarray(fm.reshape(8, 128, nch, 128).transpose(2, 1, 0, 3).reshape(nch, 128, 1024))
    tm = np.concatenate([c_kv, k_idx, w_idx, i_h], axis=1)
    w["win_tm"] = np.ascontiguousarray(tm.reshape(8, 128, 712).transpose(1, 0, 2))
    wa = inp["w_br_a"][0].reshape(4, 128, 8, 128).transpose(2, 1, 0, 3)
    wb_ = inp["w_br_b"][0].reshape(4, 128, 8, 128).transpose(2, 1, 0, 3)
    w["wbr"] = np.ascontiguousarray(np.stack([wa, wb_], axis=2).reshape(8, 128, 1024))
    w["wout"] = np.ascontiguousarray(inp["w_out"][0].reshape(8, 128, 1024).transpose(1, 0, 2))
    w["wpg"] = np.ascontiguousarray(inp["w_ple_gate"][0].reshape(8, 128, 1024).transpose(1, 0, 2))
    w["wpp"] = np.ascontiguousarray(inp["w_ple_proj"][0].reshape(2, 128, 1024).transpose(1, 0, 2))
    wuk = inp["w_uk"][0]
    a = np.zeros((128, 4, 128), np.float32)
    for h in range(8):
        a[(h % 2) * 64:(h % 2) * 64 + 64, h // 2, :] = wuk[h].T
    w["wuk"] = a
    wuv = inp["w_uv"][0]
    a = np.zeros((128, 8, 128), np.float32)
    for h in range(8):
        a[:, h, (h % 2) * 64:(h % 2) * 64 + 64] = wuv[h]
    w["wuv"] = a
    gc = np.stack([inp["ffn1_pre_g"][0], inp["mix_pre_g"][0], inp["ffn2_pre_g"][0], inp["ple_pre_g"][0]], 0)
    w["gcol"] = np.ascontiguousarray(gc.reshape(4, 8, 128).transpose(2, 0, 1))
    w["grow"] = np.ascontiguousarray(np.stack([inp["ffn1_post_g"][0], inp["mix_post_g"][0], inp["ffn2_post_g"][0], inp["ple_post_g"][0]], 0))
    w["bgate"] = np.ascontiguousarray(inp["b_gate"][0].reshape(16, 128).T)
    w["kvg"] = np.ascontiguousarray(inp["kv_norm_g"][0].reshape(1, 128))
    w["kig"] = np.ascontiguousarray(inp["kidx_norm_g"][0].reshape(1, 64))
    w["hgn"] = np.ascontiguousarray(inp["hgrn_norm_g"][0].reshape(1, 128))
    w["lbc"] = np.ascontiguousarray(inp["hgrn_lb"].reshape(2, 4, 128).transpose(2, 0, 1))
    w["relb"] = np.ascontiguousarray(inp["rel_bias"])
    return w


BIG = {"wffn1": [NFC, 128, 3072], "wffn2": [NFC, 128, 3072], "win_fm": [36, 128, 1024],
       "win_tm": [128, 8, 712], "wbr": [8, 128, 1024], "wout": [128, 8, 1024],
       "wpg": [128, 8, 1024], "wpp": [128, 2, 1024], "wuk": [128, 4, 128], "wuv": [128, 8, 128]}
SMALL = {"gcol": [128, 4, 8], "grow": [4, 1024], "bgate": [128, 16], "kvg": [1, 128], "kig": [1, 64],
         "hgn": [1, 128], "lbc": [128, 2, 4], "relb": [32, 8]}
CONST = {"c_ident": [128, 128], "c_anti": [128, 128], "c_cmneg": [128, 128], "c_cmpos": [128, 128],
         "c_tri": [128, 128], "c_oh": [32, 384], "c_halves": [128, 32]}


def build(stage=9, NIT=24, dbg=(), nm=NM):
    nc = bass.Bass("TRN2", target_bir_lowering=False)
    S = Sched(nc)
    def A(name, shape, dt):
        return nc.alloc_sbuf_tensor("sb_" + name, shape, dt)

    def din(name, shape):
        return nc.dram_tensor(name, shape, F32, kind="ExternalInput").ap()

    x_d = din("x", [SEQ, D])
    p_d = din("p", [SEQ, 256])
    big_f = {k: din(k, v) for k, v in BIG.items()}
    small_d = {k: din(k, v) for k, v in SMALL.items()}
    const_d = {k: din(k, v) for k, v in CONST.items()}
    out_d = nc.dram_tensor("out", [SEQ, D], F32, kind="ExternalOutput").ap()
    big_b = {k: nc.dram_tensor(k + "_b", v, BF16).ap() for k, v in BIG.items()}
    cst_b = {k: nc.dram_tensor(k + "_b", CONST[k], BF16).ap() for k in ("c_ident", "c_anti")}
    u_scr = nc.dram_tensor("u_scr", [8, 384], F32)
    dbg_d = {}

    def dump(name, ap, shape, keys, dt=F32):
        if name not in dbg:
            return
        key = name
        n = 0
        while key in dbg_d:
            n += 1
            key = f"{name}_{n}"
        t = nc.dram_tensor("dbg_" + key, list(shape), dt, kind="ExternalOutput").ap()
        dbg_d[key] = t
        S.dma("sp", I("dma_start", out=t, in_=ap), r=keys, w=["dbg_" + key])

    def cast(name, nsplit=None):
        src, dst = big_f[name], big_b[name]
        if nsplit is None:
            S.dma("pool", I("dma_start", out=dst, in_=src), w=[("wb", name)])
        else:
            for i in range(nsplit):
                S.dma("pool", I("dma_start", out=dst[i], in_=src[i]), w=[("wb", name, i)])

    for k in ("c_ident", "c_anti"):
        S.dma("pool", I("dma_start", out=cst_b[k], in_=const_d[k]), w=[("cb", k)])
    cast("wffn1", NFC)
    cast("wuk"); cast("wuv"); cast("win_tm")
    cast("win_fm", 36)
    cast("wbr"); cast("wout")
    cast("wffn2", NFC)
    cast("wpg"); cast("wpp")

    identb = A("identb", [128, 512], BF16)
    antib = A("antib", [128, 128], BF16)
    cmneg = A("cmneg", [128, 128], F32)
    cmpos = A("cmpos", [128, 128], F32)
    trim = A("trim", [128, 128], F32)
    halves = A("halves", [128, 32], F32)
    gcol = A("gcol", [128, 4, 8], F32)
    bgate = A("bgate", [128, 16], F32)
    bgh = A("bgh", [128, 16], F32)
    kvg = A("kvg", [128, 128], F32)
    kig = A("kig", [128, 64], F32)
    hgn = A("hgn", [128, 128], F32)
    lbc = A("lbc", [128, 2, 4], F32)
    lbk = A("lbk", [128, 4, 4], F32)
    neghalf = A("neghalf", [128, 8], F32)
    zeros64 = A("zeros64", [128, 64], F32)
    mdiag = A("mdiag", [128, 512], BF16)
    wukb = A("wukb", [128, 4, 128], BF16)
    wuvb = A("wuvb", [128, 8, 128], BF16)
    tbr = A("tbr", [128, 8, 256], BF16)
    ckvT = A("ckvT", [128, SEQ], BF16)
    ckva = A("ckva", [128, NT, 130], BF16)
    kidxT = A("kidxT", [128, SEQ], BF16)
    Sst = A("Sst", [128, 4, 128], F32)
    Sb = A("Sb", [128, 4, 128], BF16)
    st = A("st", [128, 16], F32)
    hres = A("hres", [128, 4, 1024], F32)
    xnT = A("xnT", [128, 8, 512], BF16)
    NWR = 4
    wring = A("wring", [128, NWR, 2048], BF16)
    ARENA_KB = 127
    arena = A("arena", [128, ARENA_KB * 512], BF16)
    print("sbuf bytes remaining", nc.sbuf_bytes_remaining)

    class Arena:
        def __init__(self):
            self.off = 0

        def get(self, shape, dt):
            n = int(np.prod(shape[1:]))
            nb = n * (4 if dt == F32 else (1 if dt == mybir.dt.uint8 else 2))
            nb = (nb + 63) // 64 * 64
            a = self.off // 2
            self.off += nb
            assert self.off <= ARENA_KB * 1024, self.off
            v = arena[:, a:a + nb // 2]
            if dt == F32:
                v = v.bitcast(F32)[:, 0:n]
            elif dt == mybir.dt.uint8:
                v = v.bitcast(mybir.dt.uint8)[:, 0:n]
            else:
                v = v[:, 0:n]
            if len(shape) == 3:
                v = v.rearrange("p (a b) -> p a b", a=shape[1])
            return v

    def common(ar):
        return (ar.get([128, 1024], BF16), ar.get([128, 1024], F32), [ar.get([128, 1024], BF16) for _ in range(2)],
                ar.get([128, 1024], F32))
    ar = Arena()
    junkb, ptmp, xss, growt = common(ar)
    actT = ar.get([128, NFC, 512], BF16)
    sgs = [ar.get([128, 512], F32) for _ in range(2)]
    ar = Arena()
    _j, _p, _x, _g = common(ar)
    qlm = ar.get([128, 8, 512], BF16)
    qay = ar.get([128, 4, 512], BF16)
    qidxT = ar.get([128, 4, 512], BF16)
    vtm = ar.get([128, 4, 512], BF16)
    ghT = ar.get([128, 4, 512], BF16)
    hq = [ar.get([128, 4, 512], BF16) for _ in range(4)]
    FL = ar.get([128, 4, 8], F32)
    ybT = ar.get([128, 4, 512], BF16)
    score = ar.get([128, SEQ], F32)
    Zb = ar.get([128, SEQ], BF16)
    Rr = [ar.get([128, 512], BF16) for _ in range(4)]
    PTr = [ar.get([128, 512], BF16) for _ in range(4)]
    r2_off = ar.off
    ht = [ar.get([128, 512], F32) for _ in range(6)]
    tokA = ar.get([128, 200], F32)
    ckn = ar.get([128, 128], BF16)
    kin = ar.get([128, 128], BF16)
    assert ar.off - r2_off <= SEQ * 4
    ar.off = r2_off
    score2 = ar.get([128, SEQ], F32)
    scores = [score, score2]
    R2KEYS = ["ht%d" % q for q in range(6)] + ["tokA", "ckn", ("kin", 0), ("kin", 1)]
    junk8 = ar.get([128, SEQ], mybir.dt.uint8)
    diagW = ar.get([128, 8, 128], BF16)
    smin = ar.get([128, 128], F32)
    smin2 = ar.get([128, 128], F32)
    smins = [smin, smin2]
    onb = ar.get([128, 8, 128], BF16)
    olT = ar.get([128, 8, 128], BF16)
    kdtm = ar.get([128, 4, 128], BF16)
    ATb = ar.get([128, 4, 128], BF16)
    ohn = ar.get([128, 4, 128], BF16)
    bis = ar.get([128, 64], F32)
    gtmp = [ar.get([128, 512], F32) for _ in range(2)]
    print("mixer arena bytes", ar.off)

    banks = [nc.alloc_psum_tensor(f"bank{i}", [128, 512], F32) for i in range(8)]
    ring_state = {"i": 0, "lo": 0, "hi": 8}

    def ring():
        lo, hi = ring_state["lo"], ring_state["hi"]
        i = ring_state["i"]
        if i < lo or i >= hi:
            i = lo
        ring_state["i"] = i + 1 if i + 1 < hi else lo
        return i

    def set_ring(lo, hi):
        ring_state["lo"], ring_state["hi"] = lo, hi
        ring_state["i"] = lo

    def bk(i):
        return ("bank", i)

    def ld(dst, src, keyw, keyr=()):
        S.dma("sp", I("dma_start", out=dst, in_=src), r=list(keyr), w=[keyw])

    for r_ in range(4):
        ld(identb[:, r_ * 128:(r_ + 1) * 128], cst_b["c_ident"], ("identb", r_), [("cb", "c_ident")])
    ld(antib[:], cst_b["c_anti"], "antib", [("cb", "c_anti")])
    ld(cmneg[:], const_d["c_cmneg"], "cmneg")
    ld(cmpos[:], const_d["c_cmpos"], "cmpos")
    ld(trim[:], const_d["c_tri"], "trim")
    ld(halves[:], const_d["c_halves"], "halves")
    ld(gcol[:], small_d["gcol"], "gcol")
    ld(bgate[:], small_d["bgate"], "bgate")
    ld(kvg[:], small_d["kvg"].partition_broadcast(128), "kvg")
    ld(kig[:], small_d["kig"].partition_broadcast(128), "kig")
    ld(hgn[:], small_d["hgn"].partition_broadcast(128), "hgn")
    ld(lbc[:], small_d["lbc"], "lbc")
    ld(wukb[:], big_b["wuk"], "wukb", [("wb", "wuk")])
    ld(wuvb[:], big_b["wuv"], "wuvb", [("wb", "wuv")])
    IDK = [("identb", r_) for r_ in range(4)]

    S.op("dve", I("memset", neghalf[:], -0.5), w=["neghalf"])
    S.op("dve", I("memset", zeros64[:], 0.0), w=["zeros64"])
    S.op("dve", I("memset", Sst[:], 0.0), w=["Sst"])
    S.op("dve", I("memset", Sb[:], 0.0), w=["Sb"])
    S.op("dve", I("memset", ckva[:], 1.0), w=["ckva_init"])
    S.op("dve", I("tensor_scalar", out=mdiag[:], in0=identb[:], scalar1=-1024.0, scalar2=None, op0=ALU.mult),
         r=IDK, w=["mdiag"])
    S.op("dve", I("tensor_scalar", out=bgh[:], in0=bgate[:], scalar1=0.5, scalar2=None, op0=ALU.mult),
         r=["bgate"], w=["bgh"])
    S.op("dve", I("tensor_tensor", out=lbk[:, 0, :], in0=lbc[:, 0, :], in1=lbc[:, 1, :], op=ALU.subtract),
         r=["lbc"], w=["lbk0"])
    S.op("act", I("activation", out=lbk[:, 1, :], in_=lbk[:, 0, :], func=AF.Tanh, scale=0.5), r=["lbk0"], w=["lbk1"])
    S.op("dve", I("tensor_scalar", out=lbk[:, 2, :], in0=lbk[:, 1, :], scalar1=0.25, scalar2=0.75, op0=ALU.mult, op1=ALU.add),
         r=["lbk1"], w=["c0"])
    S.op("dve", I("tensor_scalar", out=lbk[:, 3, :], in0=lbk[:, 1, :], scalar1=-0.25, scalar2=0.25, op0=ALU.mult, op1=ALU.add),
         r=["lbk1"], w=["c1"])

    ar = Arena()
    relb_s = ar.get([128, 8], F32)[0:32, :]
    oh_s = ar.get([128, 384], F32)[0:32, :]
    u_s = ar.get([128, 384], F32)[0:8, :]
    tbr32 = ar.get([128, 8, 256], F32)
    ld(relb_s[:], small_d["relb"], "relb_s")
    ld(oh_s[:], const_d["c_oh"], "oh_s")
    S.op("pe", I("matmul", banks[0][0:8, 0:384], lhsT=relb_s[:], rhs=oh_s[:], start=True, stop=True),
         r=["relb_s", "oh_s"], w=[bk(0)])
    S.op("dve", I("tensor_copy", out=u_s[:], in_=banks[0][0:8, 0:384]), r=[bk(0)], w=["u_s"])
    S.dma("sp", I("dma_start", out=u_scr.ap(), in_=u_s[:]), r=["u_s"], w=["u_scr"])
    hank = bass.AP(u_scr, 0, [[1, 128], [384, 8], [1, 256]])
    S.dma("sp", I("dma_start", out=tbr32[:], in_=hank), r=["u_scr"], w=["tbr32"])
    S.op("dve", I("tensor_copy", out=tbr[:], in_=tbr32[:]), r=["tbr32"], w=["tbr"])
    S.barrier()

    def STK(a, b=None):
        return [("st", c) for c in range(a, (a + 1) if b is None else b)]

    def rstd_from_sum(s0, d0, n, scale):
        src_ap, dst_ap = st[:, s0:s0 + n], st[:, d0:d0 + n]
        S.op("dve", I("tensor_scalar", out=dst_ap, in0=src_ap, scalar1=scale, scalar2=EPS, op0=ALU.mult, op1=ALU.add),
             r=STK(s0, s0 + n), w=STK(d0, d0 + n))
        S.op("pool", I("tensor_tensor", out=dst_ap, in0=dst_ap, in1=neghalf[:, 0:n], op=ALU.pow),
             r=STK(d0, d0 + n) + ["neghalf"], w=STK(d0, d0 + n))

    def norm_T(j, n, src_keys):
        xs = xss[j % 2]
        xk = ("xs", j % 2)
        S.op("act", I("activation", out=junkb_any[:, 0:1024], in_=hres[:, j, :], func=AF.Square, accum_out=st[:, 0:1]),
             r=src_keys, w=["junkb"] + STK(0))
        rstd_from_sum(0, 1, 1, 1.0 / D)
        S.op("dve", I("tensor_scalar", out=xs[:, :], in0=hres[:, j, :], scalar1=st[:, 1:2], scalar2=None, op0=ALU.mult),
             r=src_keys + STK(1), w=[xk])
        b = ring()
        bv = banks[b][:].bitcast(BF16).rearrange("p (a b) -> p a b", a=8)
        for dc in range(8):
            S.op("pe", I("transpose", out=bv[:, dc, :], in_=xs[:, dc * 128:(dc + 1) * 128], identity=identb[:, 0:128]),
                 r=[xk] + IDK, w=[bk(b)], inc=(dc == 7))
        S.op("dve", I("tensor_tensor", out=xnT[:, :, j * 128:(j + 1) * 128], in0=bv,
                                              in1=gcol[:, n, :].unsqueeze(2).to_broadcast([128, 8, 128]), op=ALU.mult),
             r=[bk(b), "gcol"], w=[("xnT", j)])

    wr_state = {"n": 0}

    def wload(src_ap, width, rkeys):
        s = wr_state["n"] % NWR
        wr_state["n"] += 1
        key = ("wr", s)
        S.dma("sp", I("dma_start", out=wring[:, s, 0:width], in_=src_ap), r=rkeys, w=[key])
        return wring[:, s, :], key

    XNT = [("xnT", j) for j in range(4)]

    def ffn(which, gidx_pre, gidx_post):
        wname = "wffn1" if which == 0 else "wffn2"
        wb = big_b[wname]
        set_ring(0, 8)
        load_gain(gidx_post)
        for j in range(4):
            norm_T(j, gidx_pre, [("h", j)])
        for fc in range(NFC):
            wv, wk = wload(wb[fc, :, 0:2048], 2048, [("wb", wname, fc)])
            bg, bu = ring(), ring()
            for dc in range(8):
                S.op("pe", I("matmul", banks[bg][:], lhsT=wv[:, dc * 128:(dc + 1) * 128], rhs=xnT[:, dc, :],
                                                                     start=(dc == 0), stop=(dc == 7)),
                     r=[wk] + XNT, w=[bk(bg)], inc=(dc == 7))
            for dc in range(8):
                S.op("pe", I("matmul", banks[bu][:], lhsT=wv[:, 1024 + dc * 128:1024 + (dc + 1) * 128], rhs=xnT[:, dc, :],
                                                                     start=(dc == 0), stop=(dc == 7)),
                     r=[wk] + XNT, w=[bk(bu)], inc=(dc == 7))
            sg = sgs[fc % 2]
            sk = ("sg", fc % 2)
            S.op("act", I("activation", out=sg[:, :], in_=banks[bg][:], func=AF.Silu), r=[bk(bg)], w=[sk])
            S.op("dve", I("tensor_tensor", out=actT[:, fc, :], in0=sg[:, :], in1=banks[bu][:], op=ALU.mult),
                 r=[sk, bk(bu)], w=[("actT", fc)])
        for fc in range(NFC):
            wv, wk = wload(wb[fc, :, 2048:3072], 1024, [("wb", wname, fc)])
            for j in range(4):
                for hf in range(2):
                    b = j * 2 + hf
                    S.op("pe", I("matmul", banks[b][:], lhsT=actT[:, fc, j * 128:(j + 1) * 128],
                                                                                   rhs=wv[:, hf * 512:(hf + 1) * 512],
                                                                                   start=(fc == 0), stop=(fc == NFC - 1)),
                         r=[wk, ("actT", fc)], w=[bk(b)], inc=(fc == NFC - 1 or (j == 3 and hf == 1)))
        for j in range(4):
            post_norm_residual(j, [j * 2, j * 2 + 1], gidx_post, 0.5, 1.0)

    def load_gain(gidx):
        S.dma("sp", I("dma_start", out=growt[:, :], in_=small_d["grow"][gidx:gidx + 1, :].partition_broadcast(128)),
              w=["growt"])

    def post_norm_residual(j, bks, gidx, resid_scale, yscale):
        for k, b in enumerate(bks):
            S.op("act", I("activation", out=junkb_any[:, 0:512], in_=banks[b][:], func=AF.Square, scale=yscale,
                                                          accum_out=st[:, 2 + k:3 + k]),
                 r=[bk(b)], w=["junkb"] + STK(2 + k))
        S.op("dve", I("tensor_tensor", out=st[:, 4:5], in0=st[:, 2:3], in1=st[:, 3:4], op=ALU.add),
             r=STK(2, 4), w=STK(4))
        rstd_from_sum(4, 5, 1, 1.0 / D)
        S.op("dve", I("tensor_scalar", out=st[:, 6:7], in0=st[:, 5:6], scalar1=resid_scale * yscale, scalar2=None, op0=ALU.mult),
             r=STK(5), w=STK(6))
        for k, b in enumerate(bks):
            S.op("dve", I("tensor_tensor", out=ptmp_any[:, k * 512:(k + 1) * 512], in0=banks[b][:],
                                                            in1=growt[:, k * 512:(k + 1) * 512], op=ALU.mult),
                 r=[bk(b), "growt"], w=[("ptmp", k)])
        S.op("dve", I("scalar_tensor_tensor", out=hres[:, j, :], in0=ptmp_any[:, :], scalar=st[:, 6:7], in1=hres[:, j, :],
                                                      op0=ALU.mult, op1=ALU.add),
             r=[("ptmp", 0), ("ptmp", 1), ("h", j)] + STK(6), w=[("h", j)])

    junkb_any = junkb
    ptmp_any = ptmp

    def wload3(src_ap, a, b_, rkeys):
        s_ = wr_state["n"] % NWR
        wr_state["n"] += 1
        key = ("wr", s_)
        dst = wring[:, s_, 0:a * b_].rearrange("p (a b) -> p a b", a=a)
        S.dma("sp", I("dma_start", out=dst, in_=src_ap), r=rkeys, w=[key])
        return dst, key

    def fm_proj(ch):
        wv, wk = wload(big_b["win_fm"][ch], 1024, [("wb", "win_fm", ch)])
        b = ring()
        for dc in range(8):
            S.op("pe", I("matmul", banks[b][:], lhsT=wv[:, dc * 128:(dc + 1) * 128], rhs=xnT[:, dc, :],
                         start=(dc == 0), stop=(dc == 7)), r=[wk] + XNT, w=[bk(b)], inc=(dc == 7))
        return b

    wsc4 = st
    wsc4 = A("wsc4", [128, 4, 8], F32)
    hsc = A("hsc", [128, 4, 8], F32)

    def bank3(b, a):
        return banks[b][:].rearrange("p (a b) -> p a b", a=a)

    def bankbf(b, a):
        return banks[b][:].bitcast(BF16)[:, 0:a * 128].rearrange("p (a b) -> p a b", a=a)

    def mixer_proj(m):
        set_ring(0, 8)
        load_gain(1)
        for j in range(4):
            norm_T(j, 1, [("h", j)])
        wtm = big_b["win_tm"]
        wA, wAk = wload3(wtm[:, :, 0:200], 8, 200, [("wb", "win_tm")])
        wI0, wI0k = wload3(wtm[:, 0:4, 200:712], 4, 512, [("wb", "win_tm")])
        wI1, wI1k = wload3(wtm[:, 4:8, 200:712], 4, 512, [("wb", "win_tm")])
        for j in range(4):
            i = m * 4 + j
            tsl = slice(j * 128, (j + 1) * 128)
            bA, bI = ring(), ring()
            for dc in range(8):
                S.op("pe", I("matmul", banks[bA][:, 0:200], lhsT=xnT[:, dc, tsl], rhs=wA[:, dc, :], start=(dc == 0), stop=(dc == 7)),
                     r=[wAk, ("xnT", j)], w=[bk(bA)], inc=(dc == 7))
            for dc in range(8):
                wv, wk = (wI0, wI0k) if dc < 4 else (wI1, wI1k)
                S.op("pe", I("matmul", banks[bI][:], lhsT=xnT[:, dc, tsl], rhs=wv[:, dc % 4, :], start=(dc == 0), stop=(dc == 7)),
                     r=[wk, ("xnT", j)], w=[bk(bI)], inc=(dc == 7))
            S.op("act", I("activation", out=vtm[:, j, :], in_=banks[bI][:], func=AF.Copy), r=[bk(bI)], w=["vtm"])
            S.op("dve", I("tensor_copy", out=tokA[:, :], in_=banks[bA][:, 0:200]), r=[bk(bA)], w=["tokA"])
            S.op("act", I("activation", out=junkb[:, 0:128], in_=tokA[:, 0:128], func=AF.Square, accum_out=st[:, 8:9]),
                 r=["tokA"], w=["junkb"] + STK(8))
            S.op("act", I("activation", out=junkb[:, 0:64], in_=tokA[:, 128:192], func=AF.Square, accum_out=st[:, 9:10]),
                 r=["tokA"], w=["junkb"] + STK(9))
            rstd_from_sum(8, 10, 1, 1.0 / 128)
            rstd_from_sum(9, 11, 1, 1.0 / 64)
            S.op("dve", I("scalar_tensor_tensor", out=ckn[:, :], in0=tokA[:, 0:128], scalar=st[:, 10:11], in1=kvg[:, :],
                          op0=ALU.mult, op1=ALU.mult), r=["tokA", "kvg"] + STK(10), w=["ckn"])
            S.op("act", I("activation", out=ckva[:, i, 0:128], in_=ckn[:, :], func=AF.Copy), r=["ckn", "ckva_init"], w=[("ckva", i)])
            for rep in range(2):
                S.op("dve", I("scalar_tensor_tensor", out=kin[:, rep * 64:(rep + 1) * 64], in0=tokA[:, 128:192], scalar=st[:, 11:12],
                              in1=kig[:, :], op0=ALU.mult, op1=ALU.mult), r=["tokA", "kig"] + STK(11), w=[("kin", rep)])
            S.op("dve", I("tensor_scalar", out=wsc4[:, j, :], in0=tokA[:, 192:200], scalar1=IDX_W_SCALE, scalar2=None, op0=ALU.mult),
                 r=["tokA"], w=[("wsc4", j)])
            b = ring()
            bv = bankbf(b, 2)
            S.op("pe", I("transpose", out=bv[:, 0, :], in_=ckn[:, :], identity=identb[:, 0:128]), r=["ckn"] + IDK, w=[bk(b)], inc=False)
            S.op("pe", I("transpose", out=bv[:, 1, :], in_=kin[:, :], identity=identb[:, 0:128]), r=[("kin", 0), ("kin", 1)] + IDK, w=[bk(b)])
            S.op("act", I("activation", out=ckvT[:, i * 128:(i + 1) * 128], in_=bv[:, 0, :], func=AF.Copy), r=[bk(b)], w=[("ckvT", i)])
            S.op("act", I("activation", out=kidxT[:, i * 128:(i + 1) * 128], in_=bv[:, 1, :], func=AF.Copy), r=[bk(b)], w=[("kidxT", i)])
        for c in range(4):
            b = fm_proj(c)
            S.op("act", I("activation", out=qay[:, c, :], in_=banks[b][:], func=AF.Copy), r=[bk(b)], w=["qay"])
        for c in range(4):
            b = fm_proj(4 + c)
            S.op("act", I("activation", out=qidxT[:, c, :], in_=banks[b][:], func=AF.Copy), r=[bk(b)], w=["qidxT"])
        for c in range(4):
            b = fm_proj(16 + c)
            S.op("act", I("activation", out=ghT[:, c, :], in_=banks[b][:], func=AF.Silu), r=[bk(b)], w=["ghT"])
        for h8 in range(8):
            b = ring()
            ps = slice((h8 % 2) * 64, (h8 % 2) * 64 + 64)
            S.op("pe", I("matmul", banks[b][:], lhsT=wukb[ps, h8 // 2, :], rhs=qay[ps, h8 // 2, :], start=True, stop=True),
                 r=["wukb", "qay"], w=[bk(b)])
            S.op("act", I("activation", out=qlm[:, h8, :], in_=banks[b][:], func=AF.Copy, scale=ATT_SCALE), r=[bk(b)], w=["qlm"])
        for h4 in range(4):
            bq = fm_proj(8 + h4)
            bf = fm_proj(12 + h4)
            t0, t1, t2, t3, t4, t5 = ht
            S.op("act", I("activation", out=t0[:, :], in_=banks[bq][:], func=AF.Silu), r=[bk(bq)], w=["ht0"])
            S.op("act", I("activation", out=t1[:, :], in_=banks[bf][:], func=AF.Tanh, scale=0.5), r=[bk(bf)], w=["ht1"])
            S.op("dve", I("tensor_scalar", out=t1[:, :], in0=t1[:, :], scalar1=lbk[:, 3, h4:h4 + 1], scalar2=lbk[:, 2, h4:h4 + 1],
                          op0=ALU.mult, op1=ALU.add), r=["ht1", "c0", "c1"], w=["ht1"])
            for c in range(8):
                cs = slice(c * 64, (c + 1) * 64)
                S.op("dve", I("tensor_tensor_scan", out=t2[:, cs], data0=t1[:, cs], data1=zeros64[:, :], initial=1.0,
                              op0=ALU.mult, op1=ALU.add), r=["ht1", "zeros64"], w=["ht2"])
            S.op("dve", I("reciprocal", out=t3[:, :], in_=t2[:, :]), r=["ht2"], w=["ht3"])
            S.op("dve", I("scalar_tensor_tensor", out=t4[:, :], in0=t1[:, :], scalar=1.0, in1=t3[:, :], op0=ALU.subtract, op1=ALU.mult),
                 r=["ht1", "ht3"], w=["ht4"])
            S.op("dve", I("tensor_tensor", out=t5[:, :], in0=t0[:, :], in1=t2[:, :], op=ALU.mult), r=["ht0", "ht2"], w=["ht5"])
            S.op("act", I("activation", out=hq[0][:, h4, :], in_=t5[:, :], func=AF.Copy), r=["ht5"], w=["hq0"])
            F3 = t2.rearrange("p (a b) -> p a b", a=8)
            S.op("dve", I("tensor_copy", out=FL[:, h4, :], in_=F3[:, :, 63]), r=["ht2"], w=["FL"])
            S.op("dve", I("reciprocal", out=hsc[:, 0, :], in_=F3[:, :, 31]), r=["ht2"], w=["hsc0"])
            S.op("dve", I("tensor_scalar", out=hsc[:, 1, :], in0=F3[:, :, 31], scalar1=-1.0, scalar2=None, op0=ALU.mult), r=["ht2"], w=["hsc1"])
            S.op("dve", I("tensor_scalar", out=hsc[:, 2, :], in0=F3[:, :, 63], scalar1=-1.0, scalar2=None, op0=ALU.mult), r=["ht2"], w=["hsc2"])
            t5_3 = t5.rearrange("p (a b) -> p a b", a=8)
            t4_3 = t4.rearrange("p (a b) -> p a b", a=8)
            for idx, (src3, sk, sck) in enumerate(((t5_3, "ht5", 0), (t4_3, "ht4", 1), (t4_3, "ht4", 2))):
                dst = hq[idx + 1][:, h4, :].rearrange("p (a b) -> p a b", a=8)
                S.op("dve", I("tensor_tensor", out=dst, in0=src3, in1=hsc[:, sck, :].unsqueeze(2).to_broadcast([128, 8, 64]), op=ALU.mult),
                     r=[sk, "hsc%d" % sck], w=["hq%d" % (idx + 1)])

    rr_state = {"n": 0, "p": 0}
    SH = [3, 4, 5]
    HGT = 7
    sh_state = {"i": 0}
    HGB = 6

    def shring(exclude=None):
        while True:
            b = SH[sh_state["i"] % len(SH)]
            sh_state["i"] += 1
            if b != exclude:
                return b

    def indexer_thunks(j, i, sb):
        sc, sm = scores[sb], smins[sb]
        skey, smkey = ("score", sb), ("smin", sb)
        extra = R2KEYS if sb == 1 else []
        tsl = slice(j * 128, (j + 1) * 128)
        L = (i + 1) * 128
        th = []

        def t_diag():
            S.op("dve", I("tensor_tensor", out=diagW[:, :, :], in0=identb[:, 0:128].unsqueeze(1).to_broadcast([128, 8, 128]),
                          in1=wsc4[:, j, :].unsqueeze(2).to_broadcast([128, 8, 128]), op=ALU.mult), r=IDK + [("wsc4", j)], w=["diagW"])
        th.append(t_diag)
        nk5 = (L + 511) // 512
        for kb5 in range(nk5):
            def t_blk(kb5=kb5):
                w5 = min(512, L - kb5 * 512)
                acc = shring()
                kkeys = [("kidxT", kb5 * 4 + q) for q in range((w5 + 127) // 128)]

                def dots(h8):
                    bd = shring(acc)
                    ps = slice((h8 % 2) * 64, (h8 % 2) * 64 + 64)
                    S.op("pe", I("matmul", banks[bd][:, 0:w5], lhsT=qidxT[ps, h8 // 2, tsl], rhs=kidxT[ps, kb5 * 512:kb5 * 512 + w5],
                                 start=True, stop=True), r=["qidxT"] + kkeys, w=[bk(bd)])
                    sl_ = rr_state["n"] % 4
                    rr_state["n"] += 1
                    S.op("act", I("activation", out=Rr[sl_][:, 0:w5], in_=banks[bd][:, 0:w5], func=AF.Relu), r=[bk(bd)], w=[("Rr", sl_)])
                    return sl_

                def accum(h8, sl_):
                    S.op("pe", I("matmul", banks[acc][:, 0:w5], lhsT=diagW[:, h8, :], rhs=Rr[sl_][:, 0:w5], start=(h8 == 0), stop=(h8 == 7)),
                         r=["diagW", ("Rr", sl_)], w=[bk(acc)], inc=True)
                prev = dots(0)
                for h8 in range(8):
                    nxt = dots(h8 + 1) if h8 < 7 else None
                    accum(h8, prev)
                    prev = nxt
                S.op("act", I("activation", out=sc[:, kb5 * 512:kb5 * 512 + w5], in_=banks[acc][:, 0:w5], func=AF.Copy),
                     r=[bk(acc)], w=[skey] + extra)
            th.append(t_blk)

        def t_tail():
            dsl = slice(i * 128, (i + 1) * 128)
            S.op("dve", I("tensor_tensor", out=sm[:, :], in0=sc[:, dsl], in1=cmpos[:, :], op=ALU.add), r=[skey, "cmpos"], w=[smkey])
            S.op("dve", I("tensor_tensor", out=sc[:, dsl], in0=sc[:, dsl], in1=cmneg[:, :], op=ALU.add), r=[skey, "cmneg"], w=[skey])
        th.append(t_tail)
        return th

    def merge_streams(streams):
        items = []
        for si, st_ in enumerate(streams):
            n = len(st_)
            for k, t in enumerate(st_):
                items.append(((k + 0.5) / n, si, k, t))
        items.sort(key=lambda q: (q[0], q[1], q[2]))
        return [q[3] for q in items]

    def topk_mask(j, i, sb, bg):
        sc, sm = scores[sb], smins[sb]
        skey, smkey = ("score", sb), ("smin", sb)
        L = (i + 1) * 128
        bg = list(bg)
        if i < 2:
            for t in bg:
                t()
            S.op("dve", I("tensor_scalar", out=Zb[:, 0:L], in0=sc[:, 0:L], scalar1=-20000.0, scalar2=ZEPS, op0=ALU.is_lt, op1=ALU.add),
                 r=[skey], w=["Zb"])
            return
        mx, mn1, mn2, lo, w0, mid, cnt, dl = [bis[:, q:q + 1] for q in range(8)]
        whall = bis[:, 8:8 + NIT]
        S.op("dve", I("tensor_reduce", out=mx, in_=sc[:, 0:L], axis=AX.X, op=ALU.max), r=[skey], w=["b_mx"])
        S.op("dve", I("tensor_reduce", out=mn1, in_=sc[:, 0:i * 128], axis=AX.X, op=ALU.min), r=[skey], w=["b_mn1"])
        S.op("dve", I("tensor_reduce", out=mn2, in_=sm[:, :], axis=AX.X, op=ALU.min), r=[smkey], w=["b_mn2"])
        S.op("dve", I("tensor_tensor", out=lo, in0=mn1, in1=mn2, op=ALU.min), r=["b_mn1", "b_mn2"], w=["b_lo"])
        S.op("dve", I("tensor_tensor", out=w0, in0=mx, in1=lo, op=ALU.subtract), r=["b_mx", "b_lo"], w=["b_w0"])
        S.op("dve", I("tensor_scalar", out=whall, in0=halves[:, 0:NIT], scalar1=w0, scalar2=None, op0=ALU.mult), r=["halves", "b_w0"], w=["b_wh"])
        per = (len(bg) + NIT - 1) // NIT
        for n in range(NIT):
            S.op("dve", I("tensor_tensor", out=mid, in0=whall[:, n:n + 1], in1=lo, op=ALU.add), r=["b_wh", "b_lo"], w=["b_mid"])
            S.op("dve", I("tensor_scalar", out=junk8[:, 0:L], in0=sc[:, 0:L], scalar1=mid, scalar2=None, op0=ALU.is_ge, op1=ALU.add,
                          accum_out=cnt), r=[skey, "b_mid"], w=["junk8", "b_cnt"])
            S.op("dve", I("scalar_tensor_tensor", out=dl, in0=cnt, scalar=TOPK - 0.5, in1=whall[:, n:n + 1], op0=ALU.is_ge, op1=ALU.mult),
                 r=["b_cnt", "b_wh"], w=["b_dl"])
            S.op("dve", I("tensor_tensor", out=lo, in0=lo, in1=dl, op=ALU.add), r=["b_lo", "b_dl"], w=["b_lo"])
            for _ in range(per):
                if bg:
                    bg.pop(0)()
        while bg:
            bg.pop(0)()
        S.op("dve", I("tensor_scalar", out=Zb[:, 0:L], in0=sc[:, 0:L], scalar1=lo, scalar2=ZEPS, op0=ALU.is_lt, op1=ALU.add),
             r=[skey, "b_lo"], w=["Zb"])

    OB = [(0, 0), (0, 160), (0, 320), (1, 0), (1, 160), (1, 320), (2, 0), (2, 160)]
    pt_state = {"n": 0}

    def attention_thunks(j, i):
        tsl = slice(j * 128, (j + 1) * 128)
        state = {"prev": None}
        th = []

        def logits(kb):
            slots = []
            ksl = slice(kb * 128, (kb + 1) * 128)
            near = kb >= i - 1
            for half in range(2):
                b = shring()
                b4 = bank3(b, 4)
                S.op("pe", I("matmul", b4, lhsT=ckvT[:, ksl], rhs=qlm[:, half * 4:(half + 1) * 4, tsl], start=True, stop=False),
                     r=[("ckvT", kb), "qlm"], w=[bk(b)], inc=False)
                S.op("pe", I("matmul", b4, lhsT=Zb[:, ksl], rhs=mdiag[:, :].rearrange("p (a b) -> p a b", a=4), start=False, stop=True),
                     r=["Zb", "mdiag"], w=[bk(b)], inc=(not near))
                if near:
                    off = (kb - (i - 1)) * 128
                    for hh in range(4):
                        S.op("pe", I("matmul", b4[:, hh, :], lhsT=tbr[:, half * 4 + hh, off:off + 128], rhs=antib[:, :], start=False, stop=(hh == 3),
                                     skip_group_check=True),
                             r=["tbr", "antib"], w=[bk(b)], inc=(hh == 3))
                sl_ = pt_state["n"] % 4
                pt_state["n"] += 1
                S.op("act", I("activation", out=PTr[sl_][:, :], in_=banks[b][:], func=AF.Exp), r=[bk(b)], w=[("PT", sl_)])
                slots.append(sl_)
            return slots

        def pv(kb, slots):
            for h8 in range(8):
                bnk, off = OB[h8]
                sl_ = slots[h8 // 4]
                S.op("pe", I("matmul", banks[bnk][:, off:off + 129], lhsT=PTr[sl_][:, (h8 % 4) * 128:(h8 % 4 + 1) * 128],
                             rhs=ckva[:, kb, 0:129], start=(kb == 0 and off == 0), stop=(kb == i), skip_group_check=True),
                     r=[("PT", sl_), ("ckva", kb), "ckva_init"], w=[bk(bnk)], inc=(h8 == 7))

        for kb in range(i + 1):
            def t_kb(kb=kb):
                if kb == 0:
                    state["prev"] = logits(0)
                nxt = logits(kb + 1) if kb < i else None
                pv(kb, state["prev"])
                state["prev"] = nxt
            th.append(t_kb)

        def t_norm():
            rec = st[:, 0:8]
            for h8 in range(8):
                bnk, off = OB[h8]
                S.op("dve", I("reciprocal", out=rec[:, h8:h8 + 1], in_=banks[bnk][:, off + 128:off + 129]), r=[bk(bnk)], w=STK(h8))
                S.op("dve", I("tensor_scalar", out=onb[:, h8, :], in0=banks[bnk][:, off:off + 128], scalar1=rec[:, h8:h8 + 1], scalar2=None,
                              op0=ALU.mult), r=[bk(bnk)] + STK(h8), w=[("onb", h8)])
        th.append(t_norm)

        def t_tr():
            b = shring()
            bv = bankbf(b, 8)
            for h8 in range(8):
                S.op("pe", I("transpose", out=bv[:, h8, :], in_=onb[:, h8, :], identity=identb[:, 0:128]), r=[("onb", h8)] + IDK, w=[bk(b)],
                     inc=(h8 == 7))
            S.op("act", I("activation", out=olT[:, :, :], in_=bv, func=AF.Copy), r=[bk(b)], w=["olT"])
        th.append(t_tr)

        def t_uv():
            b2 = shring()
            b24 = bank3(b2, 4)
            for c in range(4):
                for q in range(2):
                    S.op("pe", I("matmul", b24[:, c, :], lhsT=wuvb[:, 2 * c + q, :], rhs=olT[:, 2 * c + q, :], start=(q == 0), stop=(q == 1)),
                         r=["wuvb", "olT"], w=[bk(b2)], inc=(c == 3 and q == 1))
            S.op("act", I("activation", out=qay[:, :, tsl], in_=b24, func=AF.Copy), r=[bk(b2)], w=["qay"])
        th.append(t_uv)
        return th

    def hgrn_thunks(j, i):
        tsl = slice(j * 128, (j + 1) * 128)
        bo = HGB
        bo4 = bank3(bo, 4)
        th = []

        def t0():
            b = HGT
            bv = bankbf(b, 4)
            for h4 in range(4):
                S.op("pe", I("transpose", out=bv[:, h4, :], in_=hq[3][:, h4, tsl], identity=identb[:, 0:128]), r=["hq3"] + IDK, w=[bk(b)],
                     inc=(h4 == 3))
            S.op("act", I("activation", out=kdtm[:, :, :], in_=bv, func=AF.Copy), r=[bk(b)], w=["kdtm"])
            b = HGT
            b4 = bank3(b, 4)
            for h4 in range(4):
                S.op("pe", I("matmul", b4[:, h4, :], lhsT=hq[2][:, h4, tsl], rhs=hq[1][:, h4, tsl], start=True, stop=True),
                     r=["hq2", "hq1"], w=[bk(b)], inc=(h4 == 3))
            state_b["at"] = b
        state_b = {}
        th.append(t0)

        def t1():
            b = state_b["at"]
            S.op("dve", I("tensor_tensor", out=ATb[:, :, :], in0=bank3(b, 4), in1=trim[:, :].unsqueeze(1).to_broadcast([128, 4, 128]), op=ALU.mult),
                 r=[bk(b), "trim"], w=["ATb"])
        th.append(t1)
        for c in range(2):
            def t_pe(c=c):
                ps = slice(c * 64, (c + 1) * 64)
                for h4 in range(4):
                    S.op("pe", I("matmul", bo4[ps, h4, :], lhsT=hq[0][:, h4, j * 128 + c * 64:j * 128 + (c + 1) * 64], rhs=Sb[:, h4, :],
                                 start=True, stop=False), r=["hq0", "Sb"], w=[bk(bo)], inc=False)
                    S.op("pe", I("matmul", bo4[ps, h4, :], lhsT=ATb[ps, h4, c * 64:(c + 1) * 64], rhs=vtm[ps, j, h4 * 128:(h4 + 1) * 128],
                                 start=False, stop=True), r=["ATb", "vtm"], w=[bk(bo)], inc=(h4 == 3))
                bs = HGT
                bs4 = bank3(bs, 4)
                for h4 in range(4):
                    S.op("pe", I("matmul", bs4[:, h4, :], lhsT=kdtm[ps, h4, :], rhs=vtm[ps, j, h4 * 128:(h4 + 1) * 128], start=True, stop=True),
                         r=["kdtm", "vtm"], w=[bk(bs)], inc=(h4 == 3))
                state_b["bs"] = bs
            th.append(t_pe)

            def t_upd(c=c):
                bs = state_b["bs"]
                bs4 = bank3(bs, 4)
                ch = j * 2 + c
                for h4 in range(4):
                    S.op("dve", I("scalar_tensor_tensor", out=Sst[:, h4, :], in0=Sst[:, h4, :], scalar=FL[:, h4, ch:ch + 1], in1=bs4[:, h4, :],
                                  op0=ALU.mult, op1=ALU.add), r=["Sst", "FL", bk(bs)], w=["Sst"])
                S.op("act", I("activation", out=Sb[:, :, :], in_=Sst[:, :, :], func=AF.Copy), r=["Sst"], w=["Sb"])
            th.append(t_upd)

        def t_stats():
            for h4 in range(4):
                S.op("act", I("activation", out=junkb[:, 0:128], in_=bo4[:, h4, :], func=AF.Square, accum_out=st[:, 8 + h4:9 + h4]),
                     r=[bk(bo)], w=["junkb"] + STK(8 + h4))
            rstd_from_sum(8, 12, 4, 1.0 / 128)
        th.append(t_stats)

        def t_ohn():
            for h4 in range(4):
                S.op("dve", I("scalar_tensor_tensor", out=ohn[:, h4, :], in0=bo4[:, h4, :], scalar=st[:, 12 + h4:13 + h4], in1=hgn[:, :],
                              op0=ALU.mult, op1=ALU.mult), r=[bk(bo), "hgn"] + STK(12 + h4), w=["ohn"])
        th.append(t_ohn)

        def t_tr():
            b = HGT
            bv = bankbf(b, 4)
            for h4 in range(4):
                S.op("pe", I("transpose", out=bv[:, h4, :], in_=ohn[:, h4, :], identity=identb[:, 0:128]), r=["ohn"] + IDK, w=[bk(b)],
                     inc=(h4 == 3))
            state_b["tr"] = b
        th.append(t_tr)

        def t_yb():
            b = state_b["tr"]
            S.op("dve", I("tensor_tensor", out=ybT[:, :, tsl], in0=bankbf(b, 4), in1=ghT[:, :, tsl], op=ALU.mult), r=[bk(b), "ghT"], w=["ybT"])
        th.append(t_yb)
        return th

    def merge_out(m):
        set_ring(0, 8)
        g0, g1 = ht[0], ht[1]
        for oc in range(8):
            bga = fm_proj(20 + oc)
            bgb = fm_proj(28 + oc)
            wv, wk = wload(big_b["wbr"][oc], 1024, [("wb", "wbr")])
            bra, brb = ring(), ring()
            for kc in range(4):
                S.op("pe", I("matmul", banks[bra][:], lhsT=wv[:, kc * 128:(kc + 1) * 128], rhs=qay[:, kc, :], start=(kc == 0), stop=(kc == 3)),
                     r=[wk, "qay"], w=[bk(bra)], inc=(kc == 3))
            for kc in range(4):
                S.op("pe", I("matmul", banks[brb][:], lhsT=wv[:, 512 + kc * 128:512 + (kc + 1) * 128], rhs=ybT[:, kc, :], start=(kc == 0),
                             stop=(kc == 3)), r=[wk, "ybT"], w=[bk(brb)], inc=(kc == 3))
            S.op("act", I("activation", out=g0[:, :], in_=banks[bga][:], func=AF.Tanh, bias=bgh[:, oc:oc + 1], scale=0.5),
                 r=[bk(bga), "bgh"], w=["ht0", ("score", 1)])
            S.op("act", I("activation", out=g1[:, :], in_=banks[bgb][:], func=AF.Tanh, bias=bgh[:, 8 + oc:9 + oc], scale=0.5),
                 r=[bk(bgb), "bgh"], w=["ht1", ("score", 1)])
            S.op("dve", I("scalar_tensor_tensor", out=g0[:, :], in0=g0[:, :], scalar=1.0, in1=banks[bra][:], op0=ALU.add, op1=ALU.mult),
                 r=["ht0", bk(bra)], w=["ht0"])
            S.op("dve", I("scalar_tensor_tensor", out=g1[:, :], in0=g1[:, :], scalar=1.0, in1=banks[brb][:], op0=ALU.add, op1=ALU.mult),
                 r=["ht1", bk(brb)], w=["ht1"])
            S.op("dve", I("tensor_tensor", out=qlm[:, oc, :], in0=g0[:, :], in1=g1[:, :], op=ALU.add), r=["ht0", "ht1"], w=["qlm"])
        for dc in range(8):
            wv, wk = wload(big_b["wout"][:, dc, :], 1024, [("wb", "wout")])
            for j in range(4):
                for hf in range(2):
                    b = j * 2 + hf
                    S.op("pe", I("matmul", banks[b][:], lhsT=qlm[:, dc, j * 128:(j + 1) * 128], rhs=wv[:, hf * 512:(hf + 1) * 512],
                                 start=(dc == 0), stop=(dc == 7)), r=[wk, "qlm"], w=[bk(b)], inc=(dc == 7 or (j == 3 and hf == 1)))
        for j in range(4):
            post_norm_residual(j, [j * 2, j * 2 + 1], 1, 1.0, 0.5)

    ar = Arena()
    common(ar)
    ar.get([128, NFC, 512], BF16)
    [ar.get([128, 512], F32) for _ in range(2)]
    tg = ar.get([128, 4, 1024], F32)
    pT = ar.get([128, 2, 512], BF16)
    p32 = ar.get([128, 256], F32)
    p16 = ar.get([128, 256], BF16)

    def ple(m):
        set_ring(0, 8)
        load_gain(3)
        for j in range(4):
            norm_T(j, 3, [("h", j)])
        for j in range(4):
            i = m * 4 + j
            S.dma("sp", I("dma_start", out=p32[:, :], in_=p_d[i * 128:(i + 1) * 128, :]), w=["p32"])
            S.op("dve", I("tensor_copy", out=p16[:, :], in_=p32[:, :]), r=["p32"], w=["p16"])
            b = ring()
            bv = bankbf(b, 2)
            for c in range(2):
                S.op("pe", I("transpose", out=bv[:, c, :], in_=p16[:, c * 128:(c + 1) * 128], identity=identb[:, 0:128]), r=["p16"] + IDK,
                     w=[bk(b)], inc=(c == 1))
            S.op("act", I("activation", out=pT[:, :, j * 128:(j + 1) * 128], in_=bv, func=AF.Copy), r=[bk(b)], w=["pT"])
        for dc in range(8):
            wv, wk = wload(big_b["wpg"][:, dc, :], 1024, [("wb", "wpg")])
            for j in range(4):
                for hf in range(2):
                    b = j * 2 + hf
                    S.op("pe", I("matmul", banks[b][:], lhsT=xnT[:, dc, j * 128:(j + 1) * 128], rhs=wv[:, hf * 512:(hf + 1) * 512],
                                 start=(dc == 0), stop=(dc == 7)), r=[wk, ("xnT", j)], w=[bk(b)], inc=(dc == 7 or (j == 3 and hf == 1)))
        for j in range(4):
            for hf in range(2):
                b = j * 2 + hf
                S.op("act", I("activation", out=tg[:, j, hf * 512:(hf + 1) * 512], in_=banks[b][:], func=AF.Tanh, scale=0.5),
                     r=[bk(b)], w=[("tg", j, hf)])
        for dc in range(2):
            wv, wk = wload(big_b["wpp"][:, dc, :], 1024, [("wb", "wpp")])
            for j in range(4):
                for hf in range(2):
                    b = j * 2 + hf
                    S.op("pe", I("matmul", banks[b][:], lhsT=pT[:, dc, j * 128:(j + 1) * 128], rhs=wv[:, hf * 512:(hf + 1) * 512],
                                 start=(dc == 0), stop=(dc == 1)), r=[wk, "pT"], w=[bk(b)], inc=(dc == 1 or (j == 3 and hf == 1)))
        for j in range(4):
            i = m * 4 + j
            for hf in range(2):
                b = j * 2 + hf
                S.op("dve", I("scalar_tensor_tensor", out=tg[:, j, hf * 512:(hf + 1) * 512], in0=tg[:, j, hf * 512:(hf + 1) * 512], scalar=1.0,
                              in1=banks[b][:], op0=ALU.add, op1=ALU.mult), r=[("tg", j, hf), bk(b)], w=[("tg", j, hf)])
            S.op("act", I("activation", out=junkb[:, 0:1024], in_=tg[:, j, :], func=AF.Square, scale=0.5, accum_out=st[:, 4:5]),
                 r=[("tg", j, 0), ("tg", j, 1)], w=["junkb"] + STK(4))
            rstd_from_sum(4, 5, 1, 1.0 / D)
            S.op("dve", I("tensor_scalar", out=st[:, 6:7], in0=st[:, 5:6], scalar1=0.5, scalar2=None, op0=ALU.mult), r=STK(5), w=STK(6))
            S.op("dve", I("tensor_tensor", out=ptmp[:, :], in0=tg[:, j, :], in1=growt[:, :], op=ALU.mult),
                 r=[("tg", j, 0), ("tg", j, 1), "growt"], w=[("ptmp", 0), ("ptmp", 1)])
            S.op("dve", I("scalar_tensor_tensor", out=hres[:, j, :], in0=ptmp[:, :], scalar=st[:, 6:7], in1=hres[:, j, :],
                          op0=ALU.mult, op1=ALU.add), r=[("ptmp", 0), ("ptmp", 1), ("h", j)] + STK(6), w=[("h", j)])
            S.dma("sp", I("dma_start", out=out_d[i * 128:(i + 1) * 128, :], in_=hres[:, j, :]), r=[("h", j)], w=[("out", i)])

    def store_h(m):
        for j in range(4):
            i = m * 4 + j
            S.dma("sp", I("dma_start", out=out_d[i * 128:(i + 1) * 128, :], in_=hres[:, j, :]), r=[("h", j)], w=[("out", i)])

    for m in range(nm):
        for j in range(4):
            S.dma("sp", I("dma_start", out=hres[:, j, :], in_=x_d[(m * 4 + j) * 128:(m * 4 + j + 1) * 128, :]), w=[("h", j)])
        ffn(0, 0, 0)
        S.barrier()
        if stage == 1:
            store_h(m)
            S.barrier()
            continue
        mixer_proj(m)
        for t in indexer_thunks(0, m * 4, 0):
            t()
        prev_att = None
        for j in range(4):
            i = m * 4 + j
            streams = []
            if prev_att is not None:
                streams.append(prev_att)
            if j < 3:
                streams.append(indexer_thunks(j + 1, i + 1, (j + 1) % 2))
            streams.append(hgrn_thunks(j, i))
            topk_mask(j, i, j % 2, merge_streams(streams))
            prev_att = attention_thunks(j, i)
            if m == 0:
                dump("Zb", Zb[:, 0:(j + 1) * 128], [128, (j + 1) * 128], ["Zb"], BF16)
        for t in prev_att:
            t()
        if m == 0:
            dump("yaT", qay[:, :, :], [128, 4, 512], ["qay"], BF16)
            dump("ybT", ybT[:, :, :], [128, 4, 512], ["ybT"], BF16)
            dump("ckvT", ckvT[:, 0:512], [128, 512], [("ckvT", q) for q in range(4)], BF16)
            dump("kidxT", kidxT[:, 0:512], [128, 512], [("kidxT", q) for q in range(4)], BF16)
            dump("qlat", qlm[:, :, :], [128, 8, 512], ["qlm"], BF16)
        merge_out(m)
        S.barrier()
        if stage == 2:
            store_h(m)
            S.barrier()
            continue
        ffn(1, 2, 2)
        if stage == 3:
            store_h(m)
            S.barrier()
            continue
        ple(m)
        S.barrier()

    S.finish("sp")
    S.emit()
    return nc, dbg_d


_CACHE = {}


def kernel(**inputs):
    inp = {k: np.asarray(v) for k, v in inputs.items()}
    stage = int(inp.pop("_stage", 9)) if "_stage" in inp else 9
    w = host_weights(inp)
    c = host_consts()
    key = ("nc", stage)
    if key not in _CACHE:
        _CACHE[key] = build(stage=stage)
    nc, dbg_d = _CACHE[key]
    in_maps = []
    for b in range(8):
        d = {"x": np.ascontiguousarray(inp["x"][b]), "p": np.ascontiguousarray(inp["p"][0, b])}
        d.update(w)
        d.update(c)
        in_maps.append(d)
    res = run_bass_kernel_spmd(nc, in_maps, core_ids=list(range(8)))
    out = np.stack([np.asarray(r["out"]) for r in res.results], axis=0)
    return out.astype(np.float32)
```

```python
import math
import numpy as np
import concourse.bass as bass
import concourse.mybir as mybir
from concourse.bass_utils import run_bass_kernel_spmd

F32 = mybir.dt.float32
BF16 = mybir.dt.bfloat16
ALU = mybir.AluOpType
AF = mybir.ActivationFunctionType
AX = mybir.AxisListType

NDMA_RING = 8
SEQ = 4096
D = 1024
DFF = 2816
NFC = 22
EPS = 1e-6
NT = 32
NM = 8
TOPK = 256
IDX_W_SCALE = (8 ** -0.5) * (64 ** -0.5)
ATT_SCALE = 64 ** -0.5
MASKV = 30000.0
ZEPS = 2.0 ** -10


class Sched:
    ENG = ("pe", "act", "dve", "pool", "sp")

    def __init__(self, nc):
        self.nc = nc
        self.stream = {e: [] for e in self.ENG}
        self.cnt = {e: 0 for e in self.ENG}
        self.pending = {e: False for e in self.ENG}
        self.seen = {e: {} for e in self.ENG}
        self.lastw = {}
        self.readers = {}
        self.dma_n = {}
        self.dma_slot_cnt = {}
        self.sems = {}
        self.srcs = set()

    def _need(self, deps, r, w):
        for k in r:
            lw = self.lastw.get(k)
            if lw is not None:
                deps[lw[0]] = max(deps.get(lw[0], 0), lw[1])
        for k in w:
            lw = self.lastw.get(k)
            if lw is not None:
                deps[lw[0]] = max(deps.get(lw[0], 0), lw[1])
            for (s, v) in self.readers.get(k, ()):
                deps[s] = max(deps.get(s, 0), v)

    def _emit_waits(self, eng, deps):
        for s, v in deps.items():
            if eng == "pe" and s == ("e", "pe"):
                continue
            if self.seen[eng].get(s, 0) >= v:
                continue
            self.seen[eng][s] = v
            self.stream[eng].append(("wait", s, v))

    def _record(self, src, val, r, w):
        for k in r:
            self.readers.setdefault(k, []).append((src, val))
        for k in w:
            self.lastw[k] = (src, val)
            self.readers[k] = []

    def op(self, eng, fn, r=(), w=(), inc=True):
        deps = {}
        self._need(deps, r, w)
        self._emit_waits(eng, deps)
        src = ("e", eng)
        self.srcs.add(src)
        if inc:
            self.cnt[eng] += 1
            val = self.cnt[eng]
            self.pending[eng] = False
            self.stream[eng].append(("inst", fn, src, 1))
        else:
            val = self.cnt[eng] + 1
            self.pending[eng] = True
            self.stream[eng].append(("inst", fn, None, 0))
        self._record(src, val, r, w)

    def dma(self, q, fn, r=(), w=()):
        deps = {}
        self._need(deps, r, w)
        n = self.dma_n.get(q, 0)
        self.dma_n[q] = n + 1
        src = ("d", q, n % NDMA_RING)
        self.srcs.add(src)
        c = self.dma_slot_cnt.get(src, 0)
        if c > 0:
            deps[src] = max(deps.get(src, 0), 16 * c)
        self._emit_waits(q, deps)
        c += 1
        self.dma_slot_cnt[src] = c
        self.stream[q].append(("inst", fn, src, 16))
        self._record(src, 16 * c, r, w)

    def _all_latest(self):
        deps = {}
        for e in ("pe", "act", "dve", "pool"):
            if self.cnt[e] > 0:
                deps[("e", e)] = self.cnt[e]
        for src, c in self.dma_slot_cnt.items():
            deps[src] = 16 * c
        return deps

    def barrier(self):
        for e in self.ENG:
            assert not self.pending[e]
        deps = {k: v for k, v in self._all_latest().items() if k[0] == "e"}
        for e in self.ENG:
            self._emit_waits(e, dict(deps))

    def finish(self, eng="sp"):
        self._emit_waits(eng, self._all_latest())

    def emit(self):
        nc = self.nc
        from contextlib import ExitStack
        with ExitStack() as es:
            for s in sorted(self.srcs):
                self.sems[s] = es.enter_context(nc.semaphore("s_" + "_".join(map(str, s))))
            for e in self.ENG:
                assert not self.pending[e], e
            block = es.enter_context(nc.Block())
            reg = {"pe": block.tensor, "act": block.scalar, "dve": block.vector,
                   "pool": block.gpsimd, "sp": block.sync}

            def mk(e):
                def body(h):
                    pend = []
                    for it in self.stream[e]:
                        if it[0] == "wait":
                            pend.append(it)
                            continue
                        att = None
                        if pend and e in ("act", "dve", "pool") and getattr(it[1], "attach", False):
                            att = pend.pop()
                        for w_ in pend:
                            h.wait_ge(self.sems[w_[1]], w_[2])
                        pend = []
                        ins = it[1](h)
                        if att is not None:
                            ins._wait_ge(self.sems[att[1]], att[2])
                        if it[2] is not None:
                            ins.then_inc(self.sems[it[2]], it[3])
                    for w_ in pend:
                        h.wait_ge(self.sems[w_[1]], w_[2])
                return body
            for e in self.ENG:
                if self.stream[e]:
                    reg[e](mk(e))


def I(name, *a, **k):
    f = lambda h: getattr(h, name)(*a, **k)
    f.attach = (name not in ("matmul", "transpose", "dma_start")) and ("accum_out" not in k)
    return f


def _bucket_table():
    d = np.arange(0, 512, dtype=np.int32)
    max_exact = 16
    d_f = np.maximum(d, 1).astype(np.float32)
    log_b = max_exact + (np.log(d_f / np.float32(max_exact)) / np.float32(math.log(128 / max_exact))
                         * np.float32(32 - max_exact)).astype(np.int32)
    return np.where(d < max_exact, d, np.minimum(log_b, 31))


def host_consts():
    c = {}
    c["c_ident"] = np.eye(128, dtype=np.float32)
    c["c_anti"] = np.ascontiguousarray(np.eye(128, dtype=np.float32)[::-1])
    t = np.arange(128)[:, None]
    s = np.arange(128)[None, :]
    c["c_cmneg"] = np.where(s <= t, 0.0, -MASKV).astype(np.float32)
    c["c_cmpos"] = np.where(s <= t, 0.0, MASKV).astype(np.float32)
    c["c_tri"] = (((t // 64) == (s // 64)) & (t <= s)).astype(np.float32)
    bt = _bucket_table()
    oh = np.zeros((32, 384), np.float32)
    for m in range(384):
        dd = 255 - m
        if dd >= 0:
            oh[bt[dd], m] += 1.0
            oh[31, m] -= 1.0
    c["c_oh"] = oh
    nit = 32
    c["c_halves"] = np.tile((0.5 ** np.arange(1, nit + 1, dtype=np.float64)).astype(np.float32)[None, :], (128, 1))
    return c


def host_weights(inp):
    w = {}
    for n, pre in (("wffn1", "ffn1"), ("wffn2", "ffn2")):
        wg = inp[pre + "_w_gate"][0].reshape(8, 128, NFC, 128).transpose(2, 1, 0, 3).reshape(NFC, 128, 1024)
        wu = inp[pre + "_w_up"][0].reshape(8, 128, NFC, 128).transpose(2, 1, 0, 3).reshape(NFC, 128, 1024)
        wd = inp[pre + "_w_down"][0].reshape(NFC, 128, 1024)
        w[n] = np.ascontiguousarray(np.concatenate([wg, wu, wd], axis=2))
    win = inp["w_in"][0]
    offs = np.cumsum([0, 512, 128, 512, 64, 8, 512, 512, 512, 512, 1024, 1024])
    sl = lambda k: win[:, offs[k]:offs[k + 1]]
    q_a, c_kv, q_idx, k_idx, w_idx, q_h, f_h, i_h, g_h, gate_a, gate_b = [sl(k) for k in range(11)]
    fm = np.concatenate([q_a, q_idx, q_h, f_h, g_h, gate_a, gate_b], axis=1)
    nch = fm.shape[1] // 128
    w["win_fm"] = np.ascontiguousarray(fm.reshape(8, 128, nch, 128).transpose(2, 1, 0, 3).reshape(nch, 128, 1024))
    tm = np.concatenate([c_kv, k_idx, w_idx, i_h], axis=1)
    w["win_tm"] = np.ascontiguousarray(tm.reshape(8, 128, 712).transpose(1, 0, 2))
    wa = inp["w_br_a"][0].reshape(4, 128, 8, 128).transpose(2, 1, 0, 3)
    wb_ = inp["w_br_b"][0].reshape(4, 128, 8, 128).transpose(2, 1, 0, 3)
    w["wbr"] = np.ascontiguousarray(np.stack([wa, wb_], axis=2).reshape(8, 128, 1024))
    w["wout"] = np.ascontiguousarray(inp["w_out"][0].reshape(8, 128, 1024).transpose(1, 0, 2))
    w["wpg"] = np.ascontiguousarray(inp["w_ple_gate"][0].reshape(8, 128, 1024).transpose(1, 0, 2))
    w["wpp"] = np.ascontiguousarray(inp["w_ple_proj"][0].reshape(2, 128, 1024).transpose(1, 0, 2))
    wuk = inp["w_uk"][0]
    a = np.zeros((128, 4, 128), np.float32)
    for h in range(8):
        a[(h % 2) * 64:(h % 2) * 64 + 64, h // 2, :] = wuk[h].T
    w["wuk"] = a
    wuv = inp["w_uv"][0]
    a = np.zeros((128, 8, 128), np.float32)
    for h in range(8):
        a[:, h, (h % 2) * 64:(h % 2) * 64 + 64] = wuv[h]
    w["wuv"] = a
    gc = np.stack([inp["ffn1_pre_g"][0], inp["mix_pre_g"][0], inp["ffn2_pre_g"][0], inp["ple_pre_g"][0]], 0)
    w["gcol"] = np.ascontiguousarray(gc.reshape(4, 8, 128).transpose(2, 0, 1))
    w["grow"] = np.ascontiguousarray(np.stack([inp["ffn1_post_g"][0], inp["mix_post_g"][0], inp["ffn2_post_g"][0], inp["ple_post_g"][0]], 0))
    w["bgate"] = np.ascontiguousarray(inp["b_gate"][0].reshape(16, 128).T)
    w["kvg"] = np.ascontiguousarray(inp["kv_norm_g"][0].reshape(1, 128))
    w["kig"] = np.ascontiguousarray(inp["kidx_norm_g"][0].reshape(1, 64))
    w["hgn"] = np.ascontiguousarray(inp["hgrn_norm_g"][0].reshape(1, 128))
    w["lbc"] = np.ascontiguousarray(inp["hgrn_lb"].reshape(2, 4, 128).transpose(2, 0, 1))
    w["relb"] = np.ascontiguousarray(inp["rel_bias"])
    return w


BIG = {"wffn1": [NFC, 128, 3072], "wffn2": [NFC, 128, 3072], "win_fm": [36, 128, 1024],
       "win_tm": [128, 8, 712], "wbr": [8, 128, 1024], "wout": [128, 8, 1024],
       "wpg": [128, 8, 1024], "wpp": [128, 2, 1024], "wuk": [128, 4, 128], "wuv": [128, 8, 128]}
SMALL = {"gcol": [128, 4, 8], "grow": [4, 1024], "bgate": [128, 16], "kvg": [1, 128], "kig": [1, 64],
         "hgn": [1, 128], "lbc": [128, 2, 4], "relb": [32, 8]}
CONST = {"c_ident": [128, 128], "c_anti": [128, 128], "c_cmneg": [128, 128], "c_cmpos": [128, 128],
         "c_tri": [128, 128], "c_oh": [32, 384], "c_halves": [128, 32]}


def build(stage=9, NIT=24, dbg=(), nm=NM):
    nc = bass.Bass("TRN2", target_bir_lowering=False)
    S = Sched(nc)
    def A(name, shape, dt):
        return nc.alloc_sbuf_tensor("sb_" + name, shape, dt)

    def din(name, shape):
        return nc.dram_tensor(name, shape, F32, kind="ExternalInput").ap()

    x_d = din("x", [SEQ, D])
    p_d = din("p", [SEQ, 256])
    big_f = {k: din(k, v) for k, v in BIG.items()}
    small_d = {k: din(k, v) for k, v in SMALL.items()}
    const_d = {k: din(k, v) for k, v in CONST.items()}
    out_d = nc.dram_tensor("out", [SEQ, D], F32, kind="ExternalOutput").ap()
    big_b = {k: nc.dram_tensor(k + "_b", v, BF16).ap() for k, v in BIG.items()}
    cst_b = {k: nc.dram_tensor(k + "_b", CONST[k], BF16).ap() for k in ("c_ident", "c_anti")}
    u_scr = nc.dram_tensor("u_scr", [8, 384], F32)
    dbg_d = {}

    def dump(name, ap, shape, keys, dt=F32):
        if name not in dbg:
            return
        key = name
        n = 0
        while key in dbg_d:
            n += 1
            key = f"{name}_{n}"
        t = nc.dram_tensor("dbg_" + key, list(shape), dt, kind="ExternalOutput").ap()
        dbg_d[key] = t
        S.dma("sp", I("dma_start", out=t, in_=ap), r=keys, w=["dbg_" + key])

    def cast(name, nsplit=None):
        src, dst = big_f[name], big_b[name]
        if nsplit is None:
            S.dma("pool", I("dma_start", out=dst, in_=src), w=[("wb", name)])
        else:
            for i in range(nsplit):
                S.dma("pool", I("dma_start", out=dst[i], in_=src[i]), w=[("wb", name, i)])

    for k in ("c_ident", "c_anti"):
        S.dma("pool", I("dma_start", out=cst_b[k], in_=const_d[k]), w=[("cb", k)])
    cast("wffn1", NFC)
    cast("wuk"); cast("wuv"); cast("win_tm")
    cast("win_fm", 36)
    cast("wbr"); cast("wout")
    cast("wffn2", NFC)
    cast("wpg"); cast("wpp")

    identb = A("identb", [128, 512], BF16)
    antib = A("antib", [128, 128], BF16)
    cmneg = A("cmneg", [128, 128], F32)
    cmpos = A("cmpos", [128, 128], F32)
    trim = A("trim", [128, 128], F32)
    halves = A("halves", [128, 32], F32)
    gcol = A("gcol", [128, 4, 8], F32)
    bgate = A("bgate", [128, 16], F32)
    bgh = A("bgh", [128, 16], F32)
    kvg = A("kvg", [128, 128], F32)
    kig = A("kig", [128, 64], F32)
    hgn = A("hgn", [128, 128], F32)
    lbc = A("lbc", [128, 2, 4], F32)
    lbk = A("lbk", [128, 4, 4], F32)
    neghalf = A("neghalf", [128, 8], F32)
    zeros64 = A("zeros64", [128, 64], F32)
    mdiag = A("mdiag", [128, 512], BF16)
    wukb = A("wukb", [128, 4, 128], BF16)
    wuvb = A("wuvb", [128, 8, 128], BF16)
    tbr = A("tbr", [128, 8, 256], BF16)
    ckvT = A("ckvT", [128, SEQ], BF16)
    ckva = A("ckva", [128, NT, 130], BF16)
    kidxT = A("kidxT", [128, SEQ], BF16)
    Sst = A("Sst", [128, 4, 128], F32)
    Sb = A("Sb", [128, 4, 128], BF16)
    st = A("st", [128, 16], F32)
    hres = A("hres", [128, 4, 1024], F32)
    xnT = A("xnT", [128, 8, 512], BF16)
    NWR = 4
    wring = A("wring", [128, NWR, 2048], BF16)
    ARENA_KB = 127
    arena = A("arena", [128, ARENA_KB * 512], BF16)
    print("sbuf bytes remaining", nc.sbuf_bytes_remaining)

    class Arena:
        def __init__(self):
            self.off = 0

        def get(self, shape, dt):
            n = int(np.prod(shape[1:]))
            nb = n * (4 if dt == F32 else (1 if dt == mybir.dt.uint8 else 2))
            nb = (nb + 63) // 64 * 64
            a = self.off // 2
            self.off += nb
            assert self.off <= ARENA_KB * 1024, self.off
            v = arena[:, a:a + nb // 2]
            if dt == F32:
                v = v.bitcast(F32)[:, 0:n]
            elif dt == mybir.dt.uint8:
                v = v.bitcast(mybir.dt.uint8)[:, 0:n]
            else:
                v = v[:, 0:n]
            if len(shape) == 3:
                v = v.rearrange("p (a b) -> p a b", a=shape[1])
            return v

    def common(ar):
        return (ar.get([128, 1024], BF16), ar.get([128, 1024], F32), [ar.get([128, 1024], BF16) for _ in range(2)],
                ar.get([128, 1024], F32))
    ar = Arena()
    junkb, ptmp, xss, growt = common(ar)
    actT = ar.get([128, NFC, 512], BF16)
    sgs = [ar.get([128, 512], F32) for _ in range(2)]
    ar = Arena()
    _j, _p, _x, _g = common(ar)
    qlm = ar.get([128, 8, 512], BF16)
    qay = ar.get([128, 4, 512], BF16)
    qidxT = ar.get([128, 4, 512], BF16)
    vtm = ar.get([128, 4, 512], BF16)
    ghT = ar.get([128, 4, 512], BF16)
    hq = [ar.get([128, 4, 512], BF16) for _ in range(4)]
    FL = ar.get([128, 4, 8], F32)
    ybT = ar.get([128, 4, 512], BF16)
    score = ar.get([128, SEQ], F32)
    Zb = ar.get([128, SEQ], BF16)
    Rr = [ar.get([128, 512], BF16) for _ in range(4)]
    PTr = [ar.get([128, 512], BF16) for _ in range(4)]
    r2_off = ar.off
    ht = [ar.get([128, 512], F32) for _ in range(6)]
    tokA = ar.get([128, 200], F32)
    ckn = ar.get([128, 128], BF16)
    kin = ar.get([128, 128], BF16)
    assert ar.off - r2_off <= SEQ * 4
    ar.off = r2_off
    score2 = ar.get([128, SEQ], F32)
    scores = [score, score2]
    R2KEYS = ["ht%d" % q for q in range(6)] + ["tokA", "ckn", ("kin", 0), ("kin", 1)]
    junk8 = ar.get([128, SEQ], mybir.dt.uint8)
    diagW = ar.get([128, 8, 128], BF16)
    smin = ar.get([128, 128], F32)
    smin2 = ar.get([128, 128], F32)
    smins = [smin, smin2]
    onb = ar.get([128, 8, 128], BF16)
    olT = ar.get([128, 8, 128], BF16)
    kdtm = ar.get([128, 4, 128], BF16)
    ATb = ar.get([128, 4, 128], BF16)
    ohn = ar.get([128, 4, 128], BF16)
    bis = ar.get([128, 64], F32)
    gtmp = [ar.get([128, 512], F32) for _ in range(2)]
    print("mixer arena bytes", ar.off)

    banks = [nc.alloc_psum_tensor(f"bank{i}", [128, 512], F32) for i in range(8)]
    ring_state = {"i": 0, "lo": 0, "hi": 8}

    def ring():
        lo, hi = ring_state["lo"], ring_state["hi"]
        i = ring_state["i"]
        if i < lo or i >= hi:
            i = lo
        ring_state["i"] = i + 1 if i + 1 < hi else lo
        return i

    def set_ring(lo, hi):
        ring_state["lo"], ring_state["hi"] = lo, hi
        ring_state["i"] = lo

    def bk(i):
        return ("bank", i)

    def ld(dst, src, keyw, keyr=()):
        S.dma("sp", I("dma_start", out=dst, in_=src), r=list(keyr), w=[keyw])

    for r_ in range(4):
        ld(identb[:, r_ * 128:(r_ + 1) * 128], cst_b["c_ident"], ("identb", r_), [("cb", "c_ident")])
    ld(antib[:], cst_b["c_anti"], "antib", [("cb", "c_anti")])
    ld(cmneg[:], const_d["c_cmneg"], "cmneg")
    ld(cmpos[:], const_d["c_cmpos"], "cmpos")
    ld(trim[:], const_d["c_tri"], "trim")
    ld(halves[:], const_d["c_halves"], "halves")
    ld(gcol[:], small_d["gcol"], "gcol")
    ld(bgate[:], small_d["bgate"], "bgate")
    ld(kvg[:], small_d["kvg"].partition_broadcast(128), "kvg")
    ld(kig[:], small_d["kig"].partition_broadcast(128), "kig")
    ld(hgn[:], small_d["hgn"].partition_broadcast(128), "hgn")
    ld(lbc[:], small_d["lbc"], "lbc")
    ld(wukb[:], big_b["wuk"], "wukb", [("wb", "wuk")])
    ld(wuvb[:], big_b["wuv"], "wuvb", [("wb", "wuv")])
    IDK = [("identb", r_) for r_ in range(4)]

    S.op("dve", I("memset", neghalf[:], -0.5), w=["neghalf"])
    S.op("dve", I("memset", zeros64[:], 0.0), w=["zeros64"])
    S.op("dve", I("memset", Sst[:], 0.0), w=["Sst"])
    S.op("dve", I("memset", Sb[:], 0.0), w=["Sb"])
    S.op("dve", I("memset", ckva[:], 1.0), w=["ckva_init"])
    S.op("dve", I("tensor_scalar", out=mdiag[:], in0=identb[:], scalar1=-1024.0, scalar2=None, op0=ALU.mult),
         r=IDK, w=["mdiag"])
    S.op("dve", I("tensor_scalar", out=bgh[:], in0=bgate[:], scalar1=0.5, scalar2=None, op0=ALU.mult),
         r=["bgate"], w=["bgh"])
    S.op("dve", I("tensor_tensor", out=lbk[:, 0, :], in0=lbc[:, 0, :], in1=lbc[:, 1, :], op=ALU.subtract),
         r=["lbc"], w=["lbk0"])
    S.op("act", I("activation", out=lbk[:, 1, :], in_=lbk[:, 0, :], func=AF.Tanh, scale=0.5), r=["lbk0"], w=["lbk1"])
    S.op("dve", I("tensor_scalar", out=lbk[:, 2, :], in0=lbk[:, 1, :], scalar1=0.25, scalar2=0.75, op0=ALU.mult, op1=ALU.add),
         r=["lbk1"], w=["c0"])
    S.op("dve", I("tensor_scalar", out=lbk[:, 3, :], in0=lbk[:, 1, :], scalar1=-0.25, scalar2=0.25, op0=ALU.mult, op1=ALU.add),
         r=["lbk1"], w=["c1"])

    ar = Arena()
    relb_s = ar.get([128, 8], F32)[0:32, :]
    oh_s = ar.get([128, 384], F32)[0:32, :]
    u_s = ar.get([128, 384], F32)[0:8, :]
    tbr32 = ar.get([128, 8, 256], F32)
    ld(relb_s[:], small_d["relb"], "relb_s")
    ld(oh_s[:], const_d["c_oh"], "oh_s")
    S.op("pe", I("matmul", banks[0][0:8, 0:384], lhsT=relb_s[:], rhs=oh_s[:], start=True, stop=True),
         r=["relb_s", "oh_s"], w=[bk(0)])
    S.op("dve", I("tensor_copy", out=u_s[:], in_=banks[0][0:8, 0:384]), r=[bk(0)], w=["u_s"])
    S.dma("sp", I("dma_start", out=u_scr.ap(), in_=u_s[:]), r=["u_s"], w=["u_scr"])
    hank = bass.AP(u_scr, 0, [[1, 128], [384, 8], [1, 256]])
    S.dma("sp", I("dma_start", out=tbr32[:], in_=hank), r=["u_scr"], w=["tbr32"])
    S.op("dve", I("tensor_copy", out=tbr[:], in_=tbr32[:]), r=["tbr32"], w=["tbr"])
    S.barrier()

    def STK(a, b=None):
        return [("st", c) for c in range(a, (a + 1) if b is None else b)]

    def rstd_from_sum(s0, d0, n, scale):
        src_ap, dst_ap = st[:, s0:s0 + n], st[:, d0:d0 + n]
        S.op("dve", I("tensor_scalar", out=dst_ap, in0=src_ap, scalar1=scale, scalar2=EPS, op0=ALU.mult, op1=ALU.add),
             r=STK(s0, s0 + n), w=STK(d0, d0 + n))
        S.op("pool", I("tensor_tensor", out=dst_ap, in0=dst_ap, in1=neghalf[:, 0:n], op=ALU.pow),
             r=STK(d0, d0 + n) + ["neghalf"], w=STK(d0, d0 + n))

    def norm_T(j, n, src_keys):
        xs = xss[j % 2]
        xk = ("xs", j % 2)
        S.op("act", I("activation", out=junkb_any[:, 0:1024], in_=hres[:, j, :], func=AF.Square, accum_out=st[:, 0:1]),
             r=src_keys, w=["junkb"] + STK(0))
        rstd_from_sum(0, 1, 1, 1.0 / D)
        S.op("dve", I("tensor_scalar", out=xs[:, :], in0=hres[:, j, :], scalar1=st[:, 1:2], scalar2=None, op0=ALU.mult),
             r=src_keys + STK(1), w=[xk])
        b = ring()
        bv = banks[b][:].bitcast(BF16).rearrange("p (a b) -> p a b", a=8)
        for dc in range(8):
            S.op("pe", I("transpose", out=bv[:, dc, :], in_=xs[:, dc * 128:(dc + 1) * 128], identity=identb[:, 0:128]),
                 r=[xk] + IDK, w=[bk(b)], inc=(dc == 7))
        S.op("dve", I("tensor_tensor", out=xnT[:, :, j * 128:(j + 1) * 128], in0=bv,
                                              in1=gcol[:, n, :].unsqueeze(2).to_broadcast([128, 8, 128]), op=ALU.mult),
             r=[bk(b), "gcol"], w=[("xnT", j)])

    wr_state = {"n": 0}

    def wload(src_ap, width, rkeys):
        s = wr_state["n"] % NWR
        wr_state["n"] += 1
        key = ("wr", s)
        S.dma("sp", I("dma_start", out=wring[:, s, 0:width], in_=src_ap), r=rkeys, w=[key])
        return wring[:, s, :], key

    XNT = [("xnT", j) for j in range(4)]

    def ffn(which, gidx_pre, gidx_post):
        wname = "wffn1" if which == 0 else "wffn2"
        wb = big_b[wname]
        set_ring(0, 8)
        load_gain(gidx_post)
        for j in range(4):
            norm_T(j, gidx_pre, [("h", j)])
        for fc in range(NFC):
            wv, wk = wload(wb[fc, :, 0:2048], 2048, [("wb", wname, fc)])
            bg, bu = ring(), ring()
            for dc in range(8):
                S.op("pe", I("matmul", banks[bg][:], lhsT=wv[:, dc * 128:(dc + 1) * 128], rhs=xnT[:, dc, :],
                                                                     start=(dc == 0), stop=(dc == 7)),
                     r=[wk] + XNT, w=[bk(bg)], inc=(dc == 7))
            for dc in range(8):
                S.op("pe", I("matmul", banks[bu][:], lhsT=wv[:, 1024 + dc * 128:1024 + (dc + 1) * 128], rhs=xnT[:, dc, :],
                                                                     start=(dc == 0), stop=(dc == 7)),
                     r=[wk] + XNT, w=[bk(bu)], inc=(dc == 7))
            sg = sgs[fc % 2]
            sk = ("sg", fc % 2)
            S.op("act", I("activation", out=sg[:, :], in_=banks[bg][:], func=AF.Silu), r=[bk(bg)], w=[sk])
            S.op("dve", I("tensor_tensor", out=actT[:, fc, :], in0=sg[:, :], in1=banks[bu][:], op=ALU.mult),
                 r=[sk, bk(bu)], w=[("actT", fc)])
        for fc in range(NFC):
            wv, wk = wload(wb[fc, :, 2048:3072], 1024, [("wb", wname, fc)])
            for j in range(4):
                for hf in range(2):
                    b = j * 2 + hf
                    S.op("pe", I("matmul", banks[b][:], lhsT=actT[:, fc, j * 128:(j + 1) * 128],
                                                                                   rhs=wv[:, hf * 512:(hf + 1) * 512],
                                                                                   start=(fc == 0), stop=(fc == NFC - 1)),
                         r=[wk, ("actT", fc)], w=[bk(b)], inc=(fc == NFC - 1 or (j == 3 and hf == 1)))
        for j in range(4):
            post_norm_residual(j, [j * 2, j * 2 + 1], gidx_post, 0.5, 1.0)

    def load_gain(gidx):
        S.dma("sp", I("dma_start", out=growt[:, :], in_=small_d["grow"][gidx:gidx + 1, :].partition_broadcast(128)),
              w=["growt"])

    def post_norm_residual(j, bks, gidx, resid_scale, yscale):
        for k, b in enumerate(bks):
            S.op("act", I("activation", out=junkb_any[:, 0:512], in_=banks[b][:], func=AF.Square, scale=yscale,
                                                          accum_out=st[:, 2 + k:3 + k]),
                 r=[bk(b)], w=["junkb"] + STK(2 + k))
        S.op("dve", I("tensor_tensor", out=st[:, 4:5], in0=st[:, 2:3], in1=st[:, 3:4], op=ALU.add),
             r=STK(2, 4), w=STK(4))
        rstd_from_sum(4, 5, 1, 1.0 / D)
        S.op("dve", I("tensor_scalar", out=st[:, 6:7], in0=st[:, 5:6], scalar1=resid_scale * yscale, scalar2=None, op0=ALU.mult),
             r=STK(5), w=STK(6))
        for k, b in enumerate(bks):
            S.op("dve", I("tensor_tensor", out=ptmp_any[:, k * 512:(k + 1) * 512], in0=banks[b][:],
                                                            in1=growt[:, k * 512:(k + 1) * 512], op=ALU.mult),
                 r=[bk(b), "growt"], w=[("ptmp", k)])
        S.op("dve", I("scalar_tensor_tensor", out=hres[:, j, :], in0=ptmp_any[:, :], scalar=st[:, 6:7], in1=hres[:, j, :],
                                                      op0=ALU.mult, op1=ALU.add),
             r=[("ptmp", 0), ("ptmp", 1), ("h", j)] + STK(6), w=[("h", j)])

    junkb_any = junkb
    ptmp_any = ptmp

    def wload3(src_ap, a, b_, rkeys):
        s_ = wr_state["n"] % NWR
        wr_state["n"] += 1
        key = ("wr", s_)
        dst = wring[:, s_, 0:a * b_].rearrange("p (a b) -> p a b", a=a)
        S.dma("sp", I("dma_start", out=dst, in_=src_ap), r=rkeys, w=[key])
        return dst, key

    def fm_proj(ch):
        wv, wk = wload(big_b["win_fm"][ch], 1024, [("wb", "win_fm", ch)])
        b = ring()
        for dc in range(8):
            S.op("pe", I("matmul", banks[b][:], lhsT=wv[:, dc * 128:(dc + 1) * 128], rhs=xnT[:, dc, :],
                         start=(dc == 0), stop=(dc == 7)), r=[wk] + XNT, w=[bk(b)], inc=(dc == 7))
        return b

    wsc4 = st
    wsc4 = A("wsc4", [128, 4, 8], F32)
    hsc = A("hsc", [128, 4, 8], F32)

    def bank3(b, a):
        return banks[b][:].rearrange("p (a b) -> p a b", a=a)

    def bankbf(b, a):
        return banks[b][:].bitcast(BF16)[:, 0:a * 128].rearrange("p (a b) -> p a b", a=a)

    def mixer_proj(m):
        set_ring(0, 8)
        load_gain(1)
        for j in range(4):
            norm_T(j, 1, [("h", j)])
        wtm = big_b["win_tm"]
        wA, wAk = wload3(wtm[:, :, 0:200], 8, 200, [("wb", "win_tm")])
        wI0, wI0k = wload3(wtm[:, 0:4, 200:712], 4, 512, [("wb", "win_tm")])
        wI1, wI1k = wload3(wtm[:, 4:8, 200:712], 4, 512, [("wb", "win_tm")])
        for j in range(4):
            i = m * 4 + j
            tsl = slice(j * 128, (j + 1) * 128)
            bA, bI = ring(), ring()
            for dc in range(8):
                S.op("pe", I("matmul", banks[bA][:, 0:200], lhsT=xnT[:, dc, tsl], rhs=wA[:, dc, :], start=(dc == 0), stop=(dc == 7)),
                     r=[wAk, ("xnT", j)], w=[bk(bA)], inc=(dc == 7))
            for dc in range(8):
                wv, wk = (wI0, wI0k) if dc < 4 else (wI1, wI1k)
                S.op("pe", I("matmul", banks[bI][:], lhsT=xnT[:, dc, tsl], rhs=wv[:, dc % 4, :], start=(dc == 0), stop=(dc == 7)),
                     r=[wk, ("xnT", j)], w=[bk(bI)], inc=(dc == 7))
            S.op("act", I("activation", out=vtm[:, j, :], in_=banks[bI][:], func=AF.Copy), r=[bk(bI)], w=["vtm"])
            S.op("dve", I("tensor_copy", out=tokA[:, :], in_=banks[bA][:, 0:200]), r=[bk(bA)], w=["tokA"])
            S.op("act", I("activation", out=junkb[:, 0:128], in_=tokA[:, 0:128], func=AF.Square, accum_out=st[:, 8:9]),
                 r=["tokA"], w=["junkb"] + STK(8))
            S.op("act", I("activation", out=junkb[:, 0:64], in_=tokA[:, 128:192], func=AF.Square, accum_out=st[:, 9:10]),
                 r=["tokA"], w=["junkb"] + STK(9))
            rstd_from_sum(8, 10, 1, 1.0 / 128)
            rstd_from_sum(9, 11, 1, 1.0 / 64)
            S.op("dve", I("scalar_tensor_tensor", out=ckn[:, :], in0=tokA[:, 0:128], scalar=st[:, 10:11], in1=kvg[:, :],
                          op0=ALU.mult, op1=ALU.mult), r=["tokA", "kvg"] + STK(10), w=["ckn"])
            S.op("act", I("activation", out=ckva[:, i, 0:128], in_=ckn[:, :], func=AF.Copy), r=["ckn", "ckva_init"], w=[("ckva", i)])
            for rep in range(2):
                S.op("dve", I("scalar_tensor_tensor", out=kin[:, rep * 64:(rep + 1) * 64], in0=tokA[:, 128:192], scalar=st[:, 11:12],
                              in1=kig[:, :], op0=ALU.mult, op1=ALU.mult), r=["tokA", "kig"] + STK(11), w=[("kin", rep)])
            S.op("dve", I("tensor_scalar", out=wsc4[:, j, :], in0=tokA[:, 192:200], scalar1=IDX_W_SCALE, scalar2=None, op0=ALU.mult),
                 r=["tokA"], w=[("wsc4", j)])
            b = ring()
            bv = bankbf(b, 2)
            S.op("pe", I("transpose", out=bv[:, 0, :], in_=ckn[:, :], identity=identb[:, 0:128]), r=["ckn"] + IDK, w=[bk(b)], inc=False)
            S.op("pe", I("transpose", out=bv[:, 1, :], in_=kin[:, :], identity=identb[:, 0:128]), r=[("kin", 0), ("kin", 1)] + IDK, w=[bk(b)])
            S.op("act", I("activation", out=ckvT[:, i * 128:(i + 1) * 128], in_=bv[:, 0, :], func=AF.Copy), r=[bk(b)], w=[("ckvT", i)])
            S.op("act", I("activation", out=kidxT[:, i * 128:(i + 1) * 128], in_=bv[:, 1, :], func=AF.Copy), r=[bk(b)], w=[("kidxT", i)])
        for c in range(4):
            b = fm_proj(c)
            S.op("act", I("activation", out=qay[:, c, :], in_=banks[b][:], func=AF.Copy), r=[bk(b)], w=["qay"])
        for c in range(4):
            b = fm_proj(4 + c)
            S.op("act", I("activation", out=qidxT[:, c, :], in_=banks[b][:], func=AF.Copy), r=[bk(b)], w=["qidxT"])
        for c in range(4):
            b = fm_proj(16 + c)
            S.op("act", I("activation", out=ghT[:, c, :], in_=banks[b][:], func=AF.Silu), r=[bk(b)], w=["ghT"])
        for h8 in range(8):
            b = ring()
            ps = slice((h8 % 2) * 64, (h8 % 2) * 64 + 64)
            S.op("pe", I("matmul", banks[b][:], lhsT=wukb[ps, h8 // 2, :], rhs=qay[ps, h8 // 2, :], start=True, stop=True),
                 r=["wukb", "qay"], w=[bk(b)])
            S.op("act", I("activation", out=qlm[:, h8, :], in_=banks[b][:], func=AF.Copy, scale=ATT_SCALE), r=[bk(b)], w=["qlm"])
        for h4 in range(4):
            bq = fm_proj(8 + h4)
            bf = fm_proj(12 + h4)
            t0, t1, t2, t3, t4, t5 = ht
            S.op("act", I("activation", out=t0[:, :], in_=banks[bq][:], func=AF.Silu), r=[bk(bq)], w=["ht0"])
            S.op("act", I("activation", out=t1[:, :], in_=banks[bf][:], func=AF.Tanh, scale=0.5), r=[bk(bf)], w=["ht1"])
            S.op("dve", I("tensor_scalar", out=t1[:, :], in0=t1[:, :], scalar1=lbk[:, 3, h4:h4 + 1], scalar2=lbk[:, 2, h4:h4 + 1],
                          op0=ALU.mult, op1=ALU.add), r=["ht1", "c0", "c1"], w=["ht1"])
            for c in range(8):
                cs = slice(c * 64, (c + 1) * 64)
                S.op("dve", I("tensor_tensor_scan", out=t2[:, cs], data0=t1[:, cs], data1=zeros64[:, :], initial=1.0,
                              op0=ALU.mult, op1=ALU.add), r=["ht1", "zeros64"], w=["ht2"])
            S.op("dve", I("reciprocal", out=t3[:, :], in_=t2[:, :]), r=["ht2"], w=["ht3"])
            S.op("dve", I("scalar_tensor_tensor", out=t4[:, :], in0=t1[:, :], scalar=1.0, in1=t3[:, :], op0=ALU.subtract, op1=ALU.mult),
                 r=["ht1", "ht3"], w=["ht4"])
            S.op("dve", I("tensor_tensor", out=t5[:, :], in0=t0[:, :], in1=t2[:, :], op=ALU.mult), r=["ht0", "ht2"], w=["ht5"])
            S.op("act", I("activation", out=hq[0][:, h4, :], in_=t5[:, :], func=AF.Copy), r=["ht5"], w=["hq0"])
            F3 = t2.rearrange("p (a b) -> p a b", a=8)
            S.op("dve", I("tensor_copy", out=FL[:, h4, :], in_=F3[:, :, 63]), r=["ht2"], w=["FL"])
            S.op("dve", I("reciprocal", out=hsc[:, 0, :], in_=F3[:, :, 31]), r=["ht2"], w=["hsc0"])
            S.op("dve", I("tensor_scalar", out=hsc[:, 1, :], in0=F3[:, :, 31], scalar1=-1.0, scalar2=None, op0=ALU.mult), r=["ht2"], w=["hsc1"])
            S.op("dve", I("tensor_scalar", out=hsc[:, 2, :], in0=F3[:, :, 63], scalar1=-1.0, scalar2=None, op0=ALU.mult), r=["ht2"], w=["hsc2"])
            t5_3 = t5.rearrange("p (a b) -> p a b", a=8)
            t4_3 = t4.rearrange("p (a b) -> p a b", a=8)
            for idx, (src3, sk, sck) in enumerate(((t5_3, "ht5", 0), (t4_3, "ht4", 1), (t4_3, "ht4", 2))):
                dst = hq[idx + 1][:, h4, :].rearrange("p (a b) -> p a b", a=8)
                S.op("dve", I("tensor_tensor", out=dst, in0=src3, in1=hsc[:, sck, :].unsqueeze(2).to_broadcast([128, 8, 64]), op=ALU.mult),
                     r=[sk, "hsc%d" % sck], w=["hq%d" % (idx + 1)])

    rr_state = {"n": 0, "p": 0}
    SH = [3, 4, 5]
    HGT = 7
    sh_state = {"i": 0}
    HGB = 6

    def shring(exclude=None):
        while True:
            b = SH[sh_state["i"] % len(SH)]
            sh_state["i"] += 1
            if b != exclude:
                return b

    def indexer_thunks(j, i, sb):
        sc, sm = scores[sb], smins[sb]
        skey, smkey = ("score", sb), ("smin", sb)
        extra = R2KEYS if sb == 1 else []
        tsl = slice(j * 128, (j + 1) * 128)
        L = (i + 1) * 128
        th = []

        def t_diag():
            S.op("dve", I("tensor_tensor", out=diagW[:, :, :], in0=identb[:, 0:128].unsqueeze(1).to_broadcast([128, 8, 128]),
                          in1=wsc4[:, j, :].unsqueeze(2).to_broadcast([128, 8, 128]), op=ALU.mult), r=IDK + [("wsc4", j)], w=["diagW"])
        th.append(t_diag)
        nk5 = (L + 511) // 512
        for kb5 in range(nk5):
            def t_blk(kb5=kb5):
                w5 = min(512, L - kb5 * 512)
                acc = shring()
                kkeys = [("kidxT", kb5 * 4 + q) for q in range((w5 + 127) // 128)]

                def dots(h8):
                    bd = shring(acc)
                    ps = slice((h8 % 2) * 64, (h8 % 2) * 64 + 64)
                    S.op("pe", I("matmul", banks[bd][:, 0:w5], lhsT=qidxT[ps, h8 // 2, tsl], rhs=kidxT[ps, kb5 * 512:kb5 * 512 + w5],
                                 start=True, stop=True), r=["qidxT"] + kkeys, w=[bk(bd)])
                    sl_ = rr_state["n"] % 4
                    rr_state["n"] += 1
                    S.op("act", I("activation", out=Rr[sl_][:, 0:w5], in_=banks[bd][:, 0:w5], func=AF.Relu), r=[bk(bd)], w=[("Rr", sl_)])
                    return sl_

                def accum(h8, sl_):
                    S.op("pe", I("matmul", banks[acc][:, 0:w5], lhsT=diagW[:, h8, :], rhs=Rr[sl_][:, 0:w5], start=(h8 == 0), stop=(h8 == 7)),
                         r=["diagW", ("Rr", sl_)], w=[bk(acc)], inc=True)
                prev = dots(0)
                for h8 in range(8):
                    nxt = dots(h8 + 1) if h8 < 7 else None
                    accum(h8, prev)
                    prev = nxt
                S.op("act", I("activation", out=sc[:, kb5 * 512:kb5 * 512 + w5], in_=banks[acc][:, 0:w5], func=AF.Copy),
                     r=[bk(acc)], w=[skey] + extra)
            th.append(t_blk)

        def t_tail():
            dsl = slice(i * 128, (i + 1) * 128)
            S.op("dve", I("tensor_tensor", out=sm[:, :], in0=sc[:, dsl], in1=cmpos[:, :], op=ALU.add), r=[skey, "cmpos"], w=[smkey])
            S.op("dve", I("tensor_tensor", out=sc[:, dsl], in0=sc[:, dsl], in1=cmneg[:, :], op=ALU.add), r=[skey, "cmneg"], w=[skey])
        th.append(t_tail)
        return th

    def merge_streams(streams):
        items = []
        for si, st_ in enumerate(streams):
            n = len(st_)
            for k, t in enumerate(st_):
                items.append(((k + 0.5) / n, si, k, t))
        items.sort(key=lambda q: (q[0], q[1], q[2]))
        return [q[3] for q in items]

    def topk_mask(j, i, sb, bg):
        sc, sm = scores[sb], smins[sb]
        skey, smkey = ("score", sb), ("smin", sb)
        L = (i + 1) * 128
        bg = list(bg)
        if i < 2:
            for t in bg:
                t()
            S.op("dve", I("tensor_scalar", out=Zb[:, 0:L], in0=sc[:, 0:L], scalar1=-20000.0, scalar2=ZEPS, op0=ALU.is_lt, op1=ALU.add),
                 r=[skey], w=["Zb"])
            return
        mx, mn1, mn2, lo, w0, mid, cnt, dl = [bis[:, q:q + 1] for q in range(8)]
        whall = bis[:, 8:9 + NIT]
        S.op("dve", I("tensor_reduce", out=mx, in_=sc[:, 0:L], axis=AX.X, op=ALU.max), r=[skey], w=["b_mx"])
        S.op("dve", I("tensor_reduce", out=mn1, in_=sc[:, 0:i * 128], axis=AX.X, op=ALU.min), r=[skey], w=["b_mn1"])
        S.op("dve", I("tensor_reduce", out=mn2, in_=sm[:, :], axis=AX.X, op=ALU.min), r=[smkey], w=["b_mn2"])
        S.op("dve", I("tensor_tensor", out=lo, in0=mn1, in1=mn2, op=ALU.min), r=["b_mn1", "b_mn2"], w=["b_lo"])
        S.op("dve", I("tensor_tensor", out=w0, in0=mx, in1=lo, op=ALU.subtract), r=["b_mx", "b_lo"], w=["b_w0"])
        S.op("dve", I("tensor_scalar", out=whall, in0=halves[:, 0:NIT + 1], scalar1=w0, scalar2=None, op0=ALU.mult), r=["halves", "b_w0"], w=["b_wh"])
        per = (len(bg) + NIT - 1) // NIT
        S.op("dve", I("tensor_tensor", out=mid, in0=whall[:, 0:1], in1=lo, op=ALU.add), r=["b_wh", "b_lo"], w=["b_mid"])
        for n in range(NIT):
            S.op("dve", I("tensor_scalar", out=junk8[:, 0:L], in0=sc[:, 0:L], scalar1=mid, scalar2=None, op0=ALU.is_ge, op1=ALU.add,
                          accum_out=cnt), r=[skey, "b_mid"], w=["junk8", "b_cnt"])
            S.op("dve", I("tensor_scalar", out=dl, in0=cnt, scalar1=TOPK - 0.5, scalar2=0.5, op0=ALU.is_ge, op1=ALU.subtract),
                 r=["b_cnt"], w=["b_dl"])
            S.op("dve", I("scalar_tensor_tensor", out=mid, in0=dl, scalar=whall[:, n:n + 1], in1=mid, op0=ALU.mult, op1=ALU.add),
                 r=["b_dl", "b_wh", "b_mid"], w=["b_mid"])
            for _ in range(per):
                if bg:
                    bg.pop(0)()
        while bg:
            bg.pop(0)()
        S.op("dve", I("tensor_tensor", out=lo, in0=mid, in1=whall[:, NIT:NIT + 1], op=ALU.subtract), r=["b_mid", "b_wh"], w=["b_lo"])
        S.op("dve", I("tensor_scalar", out=Zb[:, 0:L], in0=sc[:, 0:L], scalar1=lo, scalar2=ZEPS, op0=ALU.is_lt, op1=ALU.add),
             r=[skey, "b_lo"], w=["Zb"])

    OB = [(0, 0), (0, 160), (0, 320), (1, 0), (1, 160), (1, 320), (2, 0), (2, 160)]
    pt_state = {"n": 0}

    def attention_thunks(j, i):
        tsl = slice(j * 128, (j + 1) * 128)
        state = {"prev": None}
        th = []

        def logits(kb):
            slots = []
            ksl = slice(kb * 128, (kb + 1) * 128)
            near = kb >= i - 1
            for half in range(2):
                b = shring()
                b4 = bank3(b, 4)
                S.op("pe", I("matmul", b4, lhsT=ckvT[:, ksl], rhs=qlm[:, half * 4:(half + 1) * 4, tsl], start=True, stop=False),
                     r=[("ckvT", kb), "qlm"], w=[bk(b)], inc=False)
                S.op("pe", I("matmul", b4, lhsT=Zb[:, ksl], rhs=mdiag[:, :].rearrange("p (a b) -> p a b", a=4), start=False, stop=True),
                     r=["Zb", "mdiag"], w=[bk(b)], inc=(not near))
                if near:
                    off = (kb - (i - 1)) * 128
                    for hh in range(4):
                        S.op("pe", I("matmul", b4[:, hh, :], lhsT=tbr[:, half * 4 + hh, off:off + 128], rhs=antib[:, :], start=False, stop=(hh == 3),
                                     skip_group_check=True),
                             r=["tbr", "antib"], w=[bk(b)], inc=(hh == 3))
                sl_ = pt_state["n"] % 4
                pt_state["n"] += 1
                S.op("act", I("activation", out=PTr[sl_][:, :], in_=banks[b][:], func=AF.Exp), r=[bk(b)], w=[("PT", sl_)])
                slots.append(sl_)
            return slots

        def pv(kb, slots):
            for h8 in range(8):
                bnk, off = OB[h8]
                sl_ = slots[h8 // 4]
                S.op("pe", I("matmul", banks[bnk][:, off:off + 129], lhsT=PTr[sl_][:, (h8 % 4) * 128:(h8 % 4 + 1) * 128],
                             rhs=ckva[:, kb, 0:129], start=(kb == 0 and off == 0), stop=(kb == i), skip_group_check=True),
                     r=[("PT", sl_), ("ckva", kb), "ckva_init"], w=[bk(bnk)], inc=(h8 == 7))

        for kb in range(i + 1):
            def t_kb(kb=kb):
                if kb == 0:
                    state["prev"] = logits(0)
                nxt = logits(kb + 1) if kb < i else None
                pv(kb, state["prev"])
                state["prev"] = nxt
            th.append(t_kb)

        def t_norm():
            rec = st[:, 0:8]
            for h8 in range(8):
                bnk, off = OB[h8]
                S.op("dve", I("reciprocal", out=rec[:, h8:h8 + 1], in_=banks[bnk][:, off + 128:off + 129]), r=[bk(bnk)], w=STK(h8))
                S.op("dve", I("tensor_scalar", out=onb[:, h8, :], in0=banks[bnk][:, off:off + 128], scalar1=rec[:, h8:h8 + 1], scalar2=None,
                              op0=ALU.mult), r=[bk(bnk)] + STK(h8), w=[("onb", h8)])
        th.append(t_norm)

        def t_tr():
            b = shring()
            bv = bankbf(b, 8)
            for h8 in range(8):
                S.op("pe", I("transpose", out=bv[:, h8, :], in_=onb[:, h8, :], identity=identb[:, 0:128]), r=[("onb", h8)] + IDK, w=[bk(b)],
                     inc=(h8 == 7))
            S.op("act", I("activation", out=olT[:, :, :], in_=bv, func=AF.Copy), r=[bk(b)], w=["olT"])
        th.append(t_tr)

        def t_uv():
            b2 = shring()
            b24 = bank3(b2, 4)
            for c in range(4):
                for q in range(2):
                    S.op("pe", I("matmul", b24[:, c, :], lhsT=wuvb[:, 2 * c + q, :], rhs=olT[:, 2 * c + q, :], start=(q == 0), stop=(q == 1)),
                         r=["wuvb", "olT"], w=[bk(b2)], inc=(c == 3 and q == 1))
            S.op("act", I("activation", out=qay[:, :, tsl], in_=b24, func=AF.Copy), r=[bk(b2)], w=["qay"])
        th.append(t_uv)
        return th

    def hgrn_thunks(j, i):
        tsl = slice(j * 128, (j + 1) * 128)
        bo = HGB
        bo4 = bank3(bo, 4)
        th = []

        def t0():
            b = HGT
            bv = bankbf(b, 4)
            for h4 in range(4):
                S.op("pe", I("transpose", out=bv[:, h4, :], in_=hq[3][:, h4, tsl], identity=identb[:, 0:128]), r=["hq3"] + IDK, w=[bk(b)],
                     inc=(h4 == 3))
            S.op("act", I("activation", out=kdtm[:, :, :], in_=bv, func=AF.Copy), r=[bk(b)], w=["kdtm"])
            b = HGT
            b4 = bank3(b, 4)
            for h4 in range(4):
                S.op("pe", I("matmul", b4[:, h4, :], lhsT=hq[2][:, h4, tsl], rhs=hq[1][:, h4, tsl], start=True, stop=True),
                     r=["hq2", "hq1"], w=[bk(b)], inc=(h4 == 3))
            state_b["at"] = b
        state_b = {}
        th.append(t0)

        def t1():
            b = state_b["at"]
            S.op("dve", I("tensor_tensor", out=ATb[:, :, :], in0=bank3(b, 4), in1=trim[:, :].unsqueeze(1).to_broadcast([128, 4, 128]), op=ALU.mult),
                 r=[bk(b), "trim"], w=["ATb"])
        th.append(t1)
        for c in range(2):
            def t_pe(c=c):
                ps = slice(c * 64, (c + 1) * 64)
                for h4 in range(4):
                    S.op("pe", I("matmul", bo4[ps, h4, :], lhsT=hq[0][:, h4, j * 128 + c * 64:j * 128 + (c + 1) * 64], rhs=Sb[:, h4, :],
                                 start=True, stop=False), r=["hq0", "Sb"], w=[bk(bo)], inc=False)
                    S.op("pe", I("matmul", bo4[ps, h4, :], lhsT=ATb[ps, h4, c * 64:(c + 1) * 64], rhs=vtm[ps, j, h4 * 128:(h4 + 1) * 128],
                                 start=False, stop=True), r=["ATb", "vtm"], w=[bk(bo)], inc=(h4 == 3))
                bs = HGT
                bs4 = bank3(bs, 4)
                for h4 in range(4):
                    S.op("pe", I("matmul", bs4[:, h4, :], lhsT=kdtm[ps, h4, :], rhs=vtm[ps, j, h4 * 128:(h4 + 1) * 128], start=True, stop=True),
                         r=["kdtm", "vtm"], w=[bk(bs)], inc=(h4 == 3))
                state_b["bs"] = bs
            th.append(t_pe)

            def t_upd(c=c):
                bs = state_b["bs"]
                bs4 = bank3(bs, 4)
                ch = j * 2 + c
                for h4 in range(4):
                    S.op("dve", I("scalar_tensor_tensor", out=Sst[:, h4, :], in0=Sst[:, h4, :], scalar=FL[:, h4, ch:ch + 1], in1=bs4[:, h4, :],
                                  op0=ALU.mult, op1=ALU.add), r=["Sst", "FL", bk(bs)], w=["Sst"])
                S.op("act", I("activation", out=Sb[:, :, :], in_=Sst[:, :, :], func=AF.Copy), r=["Sst"], w=["Sb"])
            th.append(t_upd)

        def t_stats():
            for h4 in range(4):
                S.op("act", I("activation", out=junkb[:, 0:128], in_=bo4[:, h4, :], func=AF.Square, accum_out=st[:, 8 + h4:9 + h4]),
                     r=[bk(bo)], w=["junkb"] + STK(8 + h4))
            rstd_from_sum(8, 12, 4, 1.0 / 128)
        th.append(t_stats)

        def t_ohn():
            for h4 in range(4):
                S.op("dve", I("scalar_tensor_tensor", out=ohn[:, h4, :], in0=bo4[:, h4, :], scalar=st[:, 12 + h4:13 + h4], in1=hgn[:, :],
                              op0=ALU.mult, op1=ALU.mult), r=[bk(bo), "hgn"] + STK(12 + h4), w=["ohn"])
        th.append(t_ohn)

        def t_tr():
            b = HGT
            bv = bankbf(b, 4)
            for h4 in range(4):
                S.op("pe", I("transpose", out=bv[:, h4, :], in_=ohn[:, h4, :], identity=identb[:, 0:128]), r=["ohn"] + IDK, w=[bk(b)],
                     inc=(h4 == 3))
            state_b["tr"] = b
        th.append(t_tr)

        def t_yb():
            b = state_b["tr"]
            S.op("dve", I("tensor_tensor", out=ybT[:, :, tsl], in0=bankbf(b, 4), in1=ghT[:, :, tsl], op=ALU.mult), r=[bk(b), "ghT"], w=["ybT"])
        th.append(t_yb)
        return th

    def merge_out(m):
        set_ring(0, 8)
        g0, g1 = ht[0], ht[1]
        for oc in range(8):
            bga = fm_proj(20 + oc)
            bgb = fm_proj(28 + oc)
            wv, wk = wload(big_b["wbr"][oc], 1024, [("wb", "wbr")])
            bra, brb = ring(), ring()
            for kc in range(4):
                S.op("pe", I("matmul", banks[bra][:], lhsT=wv[:, kc * 128:(kc + 1) * 128], rhs=qay[:, kc, :], start=(kc == 0), stop=(kc == 3)),
                     r=[wk, "qay"], w=[bk(bra)], inc=(kc == 3))
            for kc in range(4):
                S.op("pe", I("matmul", banks[brb][:], lhsT=wv[:, 512 + kc * 128:512 + (kc + 1) * 128], rhs=ybT[:, kc, :], start=(kc == 0),
                             stop=(kc == 3)), r=[wk, "ybT"], w=[bk(brb)], inc=(kc == 3))
            S.op("act", I("activation", out=g0[:, :], in_=banks[bga][:], func=AF.Tanh, bias=bgh[:, oc:oc + 1], scale=0.5),
                 r=[bk(bga), "bgh"], w=["ht0", ("score", 1)])
            S.op("act", I("activation", out=g1[:, :], in_=banks[bgb][:], func=AF.Tanh, bias=bgh[:, 8 + oc:9 + oc], scale=0.5),
                 r=[bk(bgb), "bgh"], w=["ht1", ("score", 1)])
            S.op("dve", I("scalar_tensor_tensor", out=g0[:, :], in0=g0[:, :], scalar=1.0, in1=banks[bra][:], op0=ALU.add, op1=ALU.mult),
                 r=["ht0", bk(bra)], w=["ht0"])
            S.op("dve", I("scalar_tensor_tensor", out=g1[:, :], in0=g1[:, :], scalar=1.0, in1=banks[brb][:], op0=ALU.add, op1=ALU.mult),
                 r=["ht1", bk(brb)], w=["ht1"])
            S.op("dve", I("tensor_tensor", out=qlm[:, oc, :], in0=g0[:, :], in1=g1[:, :], op=ALU.add), r=["ht0", "ht1"], w=["qlm"])
        for dc in range(8):
            wv, wk = wload(big_b["wout"][:, dc, :], 1024, [("wb", "wout")])
            for j in range(4):
                for hf in range(2):
                    b = j * 2 + hf
                    S.op("pe", I("matmul", banks[b][:], lhsT=qlm[:, dc, j * 128:(j + 1) * 128], rhs=wv[:, hf * 512:(hf + 1) * 512],
                                 start=(dc == 0), stop=(dc == 7)), r=[wk, "qlm"], w=[bk(b)], inc=(dc == 7 or (j == 3 and hf == 1)))
        for j in range(4):
            post_norm_residual(j, [j * 2, j * 2 + 1], 1, 1.0, 0.5)

    ar = Arena()
    common(ar)
    ar.get([128, NFC, 512], BF16)
    [ar.get([128, 512], F32) for _ in range(2)]
    tg = ar.get([128, 4, 1024], F32)
    pT = ar.get([128, 2, 512], BF16)
    p32 = ar.get([128, 256], F32)
    p16 = ar.get([128, 256], BF16)

    def ple(m):
        set_ring(0, 8)
        load_gain(3)
        for j in range(4):
            norm_T(j, 3, [("h", j)])
        for j in range(4):
            i = m * 4 + j
            S.dma("sp", I("dma_start", out=p32[:, :], in_=p_d[i * 128:(i + 1) * 128, :]), w=["p32"])
            S.op("dve", I("tensor_copy", out=p16[:, :], in_=p32[:, :]), r=["p32"], w=["p16"])
            b = ring()
            bv = bankbf(b, 2)
            for c in range(2):
                S.op("pe", I("transpose", out=bv[:, c, :], in_=p16[:, c * 128:(c + 1) * 128], identity=identb[:, 0:128]), r=["p16"] + IDK,
                     w=[bk(b)], inc=(c == 1))
            S.op("act", I("activation", out=pT[:, :, j * 128:(j + 1) * 128], in_=bv, func=AF.Copy), r=[bk(b)], w=["pT"])
        for dc in range(8):
            wv, wk = wload(big_b["wpg"][:, dc, :], 1024, [("wb", "wpg")])
            for j in range(4):
                for hf in range(2):
                    b = j * 2 + hf
                    S.op("pe", I("matmul", banks[b][:], lhsT=xnT[:, dc, j * 128:(j + 1) * 128], rhs=wv[:, hf * 512:(hf + 1) * 512],
                                 start=(dc == 0), stop=(dc == 7)), r=[wk, ("xnT", j)], w=[bk(b)], inc=(dc == 7 or (j == 3 and hf == 1)))
        for j in range(4):
            for hf in range(2):
                b = j * 2 + hf
                S.op("act", I("activation", out=tg[:, j, hf * 512:(hf + 1) * 512], in_=banks[b][:], func=AF.Tanh, scale=0.5),
                     r=[bk(b)], w=[("tg", j, hf)])
        for dc in range(2):
            wv, wk = wload(big_b["wpp"][:, dc, :], 1024, [("wb", "wpp")])
            for j in range(4):
                for hf in range(2):
                    b = j * 2 + hf
                    S.op("pe", I("matmul", banks[b][:], lhsT=pT[:, dc, j * 128:(j + 1) * 128], rhs=wv[:, hf * 512:(hf + 1) * 512],
                                 start=(dc == 0), stop=(dc == 1)), r=[wk, "pT"], w=[bk(b)], inc=(dc == 1 or (j == 3 and hf == 1)))
        for j in range(4):
            i = m * 4 + j
            for hf in range(2):
                b = j * 2 + hf
                S.op("dve", I("scalar_tensor_tensor", out=tg[:, j, hf * 512:(hf + 1) * 512], in0=tg[:, j, hf * 512:(hf + 1) * 512], scalar=1.0,
                              in1=banks[b][:], op0=ALU.add, op1=ALU.mult), r=[("tg", j, hf), bk(b)], w=[("tg", j, hf)])
            S.op("act", I("activation", out=junkb[:, 0:1024], in_=tg[:, j, :], func=AF.Square, scale=0.5, accum_out=st[:, 4:5]),
                 r=[("tg", j, 0), ("tg", j, 1)], w=["junkb"] + STK(4))
            rstd_from_sum(4, 5, 1, 1.0 / D)
            S.op("dve", I("tensor_scalar", out=st[:, 6:7], in0=st[:, 5:6], scalar1=0.5, scalar2=None, op0=ALU.mult), r=STK(5), w=STK(6))
            S.op("dve", I("tensor_tensor", out=ptmp[:, :], in0=tg[:, j, :], in1=growt[:, :], op=ALU.mult),
                 r=[("tg", j, 0), ("tg", j, 1), "growt"], w=[("ptmp", 0), ("ptmp", 1)])
            S.op("dve", I("scalar_tensor_tensor", out=hres[:, j, :], in0=ptmp[:, :], scalar=st[:, 6:7], in1=hres[:, j, :],
                          op0=ALU.mult, op1=ALU.add), r=[("ptmp", 0), ("ptmp", 1), ("h", j)] + STK(6), w=[("h", j)])
            S.dma("sp", I("dma_start", out=out_d[i * 128:(i + 1) * 128, :], in_=hres[:, j, :]), r=[("h", j)], w=[("out", i)])

    def store_h(m):
        for j in range(4):
            i = m * 4 + j
            S.dma("sp", I("dma_start", out=out_d[i * 128:(i + 1) * 128, :], in_=hres[:, j, :]), r=[("h", j)], w=[("out", i)])

    for m in range(nm):
        for j in range(4):
            S.dma("sp", I("dma_start", out=hres[:, j, :], in_=x_d[(m * 4 + j) * 128:(m * 4 + j + 1) * 128, :]), w=[("h", j)])
        ffn(0, 0, 0)
        S.barrier()
        if stage == 1:
            store_h(m)
            S.barrier()
            continue
        mixer_proj(m)
        for t in indexer_thunks(0, m * 4, 0):
            t()
        prev_att = None
        for j in range(4):
            i = m * 4 + j
            streams = []
            if prev_att is not None:
                streams.append(prev_att)
            if j < 3:
                streams.append(indexer_thunks(j + 1, i + 1, (j + 1) % 2))
            streams.append(hgrn_thunks(j, i))
            topk_mask(j, i, j % 2, merge_streams(streams))
            prev_att = attention_thunks(j, i)
            if m == 0:
                dump("Zb", Zb[:, 0:(j + 1) * 128], [128, (j + 1) * 128], ["Zb"], BF16)
        for t in prev_att:
            t()
        if m == 0:
            dump("yaT", qay[:, :, :], [128, 4, 512], ["qay"], BF16)
            dump("ybT", ybT[:, :, :], [128, 4, 512], ["ybT"], BF16)
            dump("ckvT", ckvT[:, 0:512], [128, 512], [("ckvT", q) for q in range(4)], BF16)
            dump("kidxT", kidxT[:, 0:512], [128, 512], [("kidxT", q) for q in range(4)], BF16)
            dump("qlat", qlm[:, :, :], [128, 8, 512], ["qlm"], BF16)
        merge_out(m)
        S.barrier()
        if stage == 2:
            store_h(m)
            S.barrier()
            continue
        ffn(1, 2, 2)
        if stage == 3:
            store_h(m)
            S.barrier()
            continue
        ple(m)
        S.barrier()

    S.finish("sp")
    S.emit()
    return nc, dbg_d


_CACHE = {}


def kernel(**inputs):
    inp = {k: np.asarray(v) for k, v in inputs.items()}
    stage = int(inp.pop("_stage", 9)) if "_stage" in inp else 9
    w = host_weights(inp)
    c = host_consts()
    key = ("nc", stage)
    if key not in _CACHE:
        _CACHE[key] = build(stage=stage)
    nc, dbg_d = _CACHE[key]
    in_maps = []
    for b in range(8):
        d = {"x": np.ascontiguousarray(inp["x"][b]), "p": np.ascontiguousarray(inp["p"][0, b])}
        d.update(w)
        d.update(c)
        in_maps.append(d)
    res = run_bass_kernel_spmd(nc, in_maps, core_ids=list(range(8)))
    out = np.stack([np.asarray(r["out"]) for r in res.results], axis=0)
    return out.astype(np.float32)
```
